# Optimizing a Trainium2 kernel written in Bass

```python
import jax, jax.numpy as jnp
from jax import lax
import numpy as np

D_MODEL = 1024
BATCH = 8
SEQ = 2048
DEPTH = 2

GRID_W = 64
N_MIXERS = 2
HEAD_DIM = 64
NA_HEADS = 16
NA_WIN_H = 8
NA_WIN_W = 16
NA_QCOL_BLOCK = 16
NA_KCOL_SPAN = NA_QCOL_BLOCK + NA_WIN_W
SW_Q_HEADS = 16
SW_KV_HEADS = 4
SW_GROUP = SW_Q_HEADS // SW_KV_HEADS
SW_WINDOW = 128
SW_BLOCK = 128
T5_BUCKETS = 32
T5_MAX_DIST = 128
N_EXPERTS = 16
EXPERT_FF = 2048
EC_CAPACITY = 2
RMS_EPS = 1e-6
NEG = -1e30

kernel_name = "hybrid_natten_swa_ec_moe_encoder"


def rms_norm(x, g):
    x32 = x.astype(jnp.float32)
    y = x32 * lax.rsqrt(jnp.mean(x32 * x32, axis=-1, keepdims=True) + RMS_EPS)
    return (y * g.astype(jnp.float32)).astype(x.dtype)


def neighbourhood_attention(h, w_qkv, w_o, rpb):
    B, S, _ = h.shape
    rows = S // GRID_W
    kh = min(NA_WIN_H, rows)
    ncb = GRID_W // NA_QCOL_BLOCK
    q, k, v = jnp.split(h @ w_qkv, 3, axis=-1)
    grid = lambda t: t.reshape(B, rows, GRID_W, NA_HEADS, HEAD_DIM)
    q, k, v = grid(q) * (HEAD_DIM ** -0.5), grid(k), grid(v)
    qcol = np.arange(GRID_W).reshape(ncb, NA_QCOL_BLOCK)
    cstart = np.clip(qcol - NA_WIN_W // 2, 0, GRID_W - NA_WIN_W)
    kstart = np.clip(np.arange(ncb) * NA_QCOL_BLOCK - NA_WIN_W // 2, 0, GRID_W - NA_KCOL_SPAN)
    kcol = kstart[:, None] + np.arange(NA_KCOL_SPAN)
    col_ok = (kcol[:, None, :] >= cstart[:, :, None]) & (kcol[:, None, :] < cstart[:, :, None] + NA_WIN_W)
    dc_idx = np.clip(kcol[:, None, :] - qcol[:, :, None] + NA_WIN_W - 1, 0, 2 * NA_WIN_W - 2)
    mask = col_ok[:, :, None, :]

    def row_step(r):
        rs = jnp.clip(r - kh // 2, 0, rows - kh)
        q_b = lax.dynamic_index_in_dim(q, r, axis=1, keepdims=False).reshape(
            B, ncb, NA_QCOL_BLOCK, NA_HEADS, HEAD_DIM)
        k_b = lax.dynamic_slice_in_dim(k, rs, kh, axis=1)[:, :, kcol]
        v_b = lax.dynamic_slice_in_dim(v, rs, kh, axis=1)[:, :, kcol]
        s = jnp.einsum('bnqhd,binchd->bhnqic', q_b, k_b).astype(jnp.float32)
        dr_idx = rs + jnp.arange(kh) - r + NA_WIN_H - 1
        bias = rpb[:, dr_idx][:, :, dc_idx].astype(jnp.float32)
        bias = jnp.transpose(bias, (0, 2, 3, 1, 4))
        s = jnp.where(mask, s + bias[None], NEG)
        p = jax.nn.softmax(s.reshape(s.shape[:4] + (-1,)), axis=-1).reshape(s.shape)
        o = jnp.einsum('bhnqic,binchd->bnqhd', p.astype(v.dtype), v_b)
        return o.reshape(B, GRID_W, NA_HEADS * HEAD_DIM)

    out = lax.map(row_step, jnp.arange(rows))
    return jnp.moveaxis(out, 0, 1).reshape(B, S, NA_HEADS * HEAD_DIM) @ w_o


def t5_buckets(rel):
    half = T5_BUCKETS // 2
    max_exact = half // 2
    n = np.abs(rel)
    large = max_exact + (np.log(np.maximum(n, 1) / max_exact)
                         / np.log(T5_MAX_DIST / max_exact) * (half - max_exact)).astype(np.int32)
    large = np.minimum(large, half - 1)
    return (rel > 0).astype(np.int32) * half + np.where(n < max_exact, n, large)


def sliding_window_gqa(h, w_qkv, w_o, sinks, t5_table):
    B, S, _ = h.shape
    nb = S // SW_BLOCK
    qkv = h @ w_qkv
    nq = SW_Q_HEADS * HEAD_DIM
    nkv = SW_KV_HEADS * HEAD_DIM
    q = qkv[..., :nq].reshape(B, nb, SW_BLOCK, SW_KV_HEADS, SW_GROUP, HEAD_DIM) * (HEAD_DIM ** -0.5)
    k = qkv[..., nq:nq + nkv].reshape(B, S, SW_KV_HEADS, HEAD_DIM)
    v = qkv[..., nq + nkv:].reshape(B, S, SW_KV_HEADS, HEAD_DIM)

    def band(t):
        tp = jnp.pad(t, ((0, 0), (SW_BLOCK, SW_BLOCK), (0, 0), (0, 0))).reshape(
            B, nb + 2, SW_BLOCK, SW_KV_HEADS, HEAD_DIM)
        return jnp.concatenate([tp[:, :-2], tp[:, 1:-1], tp[:, 2:]], axis=2)

    kb, vb = band(k), band(v)
    rel = (np.arange(3 * SW_BLOCK)[None, :] - SW_BLOCK) - np.arange(SW_BLOCK)[:, None]
    kpos = np.arange(nb)[:, None] * SW_BLOCK - SW_BLOCK + np.arange(3 * SW_BLOCK)[None, :]
    mask = (np.abs(rel) <= SW_WINDOW)[None] & ((kpos >= 0) & (kpos < S))[:, None, :]
    bias = jnp.transpose(t5_table[t5_buckets(rel)].astype(jnp.float32), (2, 0, 1))
    bias = bias.reshape(SW_KV_HEADS, SW_GROUP, 1, SW_BLOCK, 3 * SW_BLOCK)
    s = jnp.einsum('bnqkgd,bnjkd->bkgnqj', q, kb).astype(jnp.float32)
    s = jnp.where(mask, s + bias, NEG)
    sink = sinks.astype(jnp.float32).reshape(1, SW_KV_HEADS, SW_GROUP, 1, 1, 1)
    m = jnp.maximum(jnp.max(s, axis=-1, keepdims=True), sink)
    p = jnp.exp(s - m)
    p = p / (jnp.sum(p, axis=-1, keepdims=True) + jnp.exp(sink - m))
    o = jnp.einsum('bkgnqj,bnjkd->bnqkgd', p.astype(v.dtype), vb)
    return o.reshape(B, S, nq) @ w_o


def expert_choice_ffn(h, w_router, w_gate, w_up, w_down):
    B, S, D = h.shape
    cap = EC_CAPACITY * S // N_EXPERTS
    aff = jax.nn.softmax((h @ w_router).astype(jnp.float32), axis=-1)
    g, idx = lax.top_k(jnp.swapaxes(aff, 1, 2), cap)
    xg = jax.vmap(lambda hb, ib: hb[ib])(h, idx)
    a = jnp.einsum('becd,edf->becf', xg, w_gate)
    u = jnp.einsum('becd,edf->becf', xg, w_up)
    y = jnp.einsum('becf,efd->becd', jax.nn.silu(a) * u, w_down) * g[..., None].astype(h.dtype)
    return jax.vmap(lambda yb, ib: jnp.zeros((S, D), h.dtype).at[ib.reshape(-1)].add(
        yb.reshape(-1, D)))(y, idx)


def setup_inputs(seed: int = 0) -> dict:
    key = jax.random.key(seed)
    ks = jax.random.split(key, 17)
    D = D_MODEL
    n_a = (DEPTH + N_MIXERS - 1) // N_MIXERS
    n_b = DEPTH // N_MIXERS
    nrm = lambda k, shape, s: jax.random.normal(k, shape, jnp.float32) * s
    sw_width = (SW_Q_HEADS + 2 * SW_KV_HEADS) * HEAD_DIM
    return {
        "x": nrm(ks[0], (BATCH, SEQ, D), 1.0),
        "c": nrm(ks[1], (BATCH, D), 1.0),
        "ada_w": nrm(ks[2], (DEPTH, D, 6 * D), 0.5 * D ** -0.5),
        "ada_b": nrm(ks[3], (DEPTH, 6 * D), 0.01),
        "norm_g": 1.0 + nrm(ks[4], (DEPTH, 2, D), 0.01),
        "na_w_qkv": nrm(ks[5], (n_a, D, 3 * NA_HEADS * HEAD_DIM), D ** -0.5),
        "na_w_o": nrm(ks[6], (n_a, NA_HEADS * HEAD_DIM, D), (NA_HEADS * HEAD_DIM) ** -0.5),
        "na_rpb": nrm(ks[7], (n_a, NA_HEADS, 2 * NA_WIN_H - 1, 2 * NA_WIN_W - 1), 0.1),
        "sw_w_qkv": nrm(ks[8], (n_b, D, sw_width), D ** -0.5),
        "sw_w_o": nrm(ks[9], (n_b, SW_Q_HEADS * HEAD_DIM, D), (SW_Q_HEADS * HEAD_DIM) ** -0.5),
        "sw_sinks": nrm(ks[10], (n_b, SW_Q_HEADS), 0.5),
        "t5_bias": nrm(ks[11], (T5_BUCKETS, SW_Q_HEADS), 0.1),
        "moe_w_router": nrm(ks[12], (DEPTH, D, N_EXPERTS), D ** -0.5),
        "moe_w_gate": nrm(ks[13], (DEPTH, N_EXPERTS, D, EXPERT_FF), D ** -0.5),
        "moe_w_up": nrm(ks[14], (DEPTH, N_EXPERTS, D, EXPERT_FF), D ** -0.5),
        "moe_w_down": nrm(ks[15], (DEPTH, N_EXPERTS, EXPERT_FF, D), EXPERT_FF ** -0.5),
        "final_g": 1.0 + nrm(ks[16], (D,), 0.01),
    }


def reference(x, c, ada_w, ada_b, norm_g, na_w_qkv, na_w_o, na_rpb, sw_w_qkv, sw_w_o,
              sw_sinks, t5_bias, moe_w_router, moe_w_gate, moe_w_up, moe_w_down, final_g):
    c_act = jax.nn.silu(c)
    for l in range(DEPTH):
        mod = c_act @ ada_w[l] + ada_b[l]
        sh1, sc1, g1, sh2, sc2, g2 = jnp.split(mod, 6, axis=-1)
        h = rms_norm(x, norm_g[l, 0]) * (1.0 + sc1[:, None]) + sh1[:, None]
        j = l // N_MIXERS
        if l % N_MIXERS == 0:
            y = neighbourhood_attention(h, na_w_qkv[j], na_w_o[j], na_rpb[j])
        else:
            y = sliding_window_gqa(h, sw_w_qkv[j], sw_w_o[j], sw_sinks[j], t5_bias)
        x = x + g1[:, None] * y
        h = rms_norm(x, norm_g[l, 1]) * (1.0 + sc2[:, None]) + sh2[:, None]
        x = x + g2[:, None] * expert_choice_ffn(h, moe_w_router[l], moe_w_gate[l],
                                                moe_w_up[l], moe_w_down[l])
    return rms_norm(x, final_g)
```

```python
import numpy as np
from contextlib import ExitStack
import concourse.bass as bass
import concourse.mybir as mybir
from concourse.bass_utils import run_bass_kernel_spmd

F32 = mybir.dt.float32
BF16 = mybir.dt.bfloat16
U32 = mybir.dt.uint32
AF = mybir.ActivationFunctionType
ALU = mybir.AluOpType
AX = mybir.AxisListType

S_LEN = 2048
D = 1024
NT = 16
KC = 8
NE = 16
FF = 2048
CAP = 256
EPS = 1e-6
NEG = -1e30
NA_TW = [512, 512, 576, 512, 512]
NA_TOFF = [0, 512, 1024, 1600, 2112]
NA_TTOT = 2624


class Sched:
    ENG = ("pe", "act", "dve", "pool", "sp")

    def __init__(self, nc, n_dma_sems=8):
        self.nc = nc
        self.eng = {"pe": nc.tensor, "act": nc.scalar, "dve": nc.vector,
                    "pool": nc.gpsimd, "sp": nc.sync}
        self._ctx = []
        self.csem = {}
        self.ccount = {e: 0 for e in self.ENG}
        for e in self.ENG:
            self.csem[e] = self._sem("c_" + e)
        self.dsem = {}
        self.dcount = {}
        self.dnext = {}
        for q in ("sp", "act", "pool"):
            self.dsem[q] = [self._sem("d_%s%d" % (q, i)) for i in range(n_dma_sems)]
            self.dcount[q] = [0] * n_dma_sems
            self.dnext[q] = 0
        self.last_write = {}
        self.readers = {}
        self.seen = {e: {} for e in self.ENG}
        self.n_wait = 0
        self.n_ops = 0

    def _sem(self, name):
        cm = self.nc.semaphore(name)
        s = cm.__enter__()
        self._ctx.append(cm)
        return s

    def _need(self, e, tok):
        kind, key, val, sem, src = tok
        if kind == "c" and src == e and e == "pe":
            return
        k = (kind, key)
        if self.seen[e].get(k, 0) >= val:
            return
        self.seen[e][k] = val
        self.eng[e].wait_ge(sem, val)
        self.n_wait += 1

    def _deps(self, e, reads, writes):
        toks = []
        for r in reads:
            t = self.last_write.get(r)
            if t is not None:
                toks.append(t)
        for w in writes:
            t = self.last_write.get(w)
            if t is not None:
                toks.append(t)
            toks.extend(self.readers.get(w, ()))
        for t in toks:
            self._need(e, t)

    def _commit(self, tok, reads, writes):
        for r in reads:
            self.readers.setdefault(r, []).append(tok)
        for w in writes:
            self.last_write[w] = tok
            self.readers[w] = []

    def op(self, e, fn, reads=(), writes=()):
        self._deps(e, reads, writes)
        ins = fn(self.eng[e])
        self.ccount[e] += 1
        ins.then_inc(self.csem[e], 1)
        tok = ("c", e, self.ccount[e], self.csem[e], e)
        self._commit(tok, reads, writes)
        self.n_ops += 1
        return tok

    def dma(self, q, fn, reads=(), writes=()):
        i = self.dnext[q]
        self.dnext[q] = (i + 1) % len(self.dsem[q])
        sem = self.dsem[q][i]
        if self.dcount[q][i] > 0:
            self._need(q, ("d", (q, i), self.dcount[q][i] * 16, sem, q))
        self._deps(q, reads, writes)
        ins = fn(self.eng[q])
        self.dcount[q][i] += 1
        ins.then_inc(sem, 16)
        tok = ("d", (q, i), self.dcount[q][i] * 16, sem, q)
        self._commit(tok, reads, writes)
        self.n_ops += 1
        return tok

    def wait_all_on(self, e):
        for f in self.ENG:
            if self.ccount[f] > 0:
                self._need(e, ("c", f, self.ccount[f], self.csem[f], f))
        for q in self.dsem:
            for i, sem in enumerate(self.dsem[q]):
                if self.dcount[q][i] > 0:
                    self._need(e, ("d", (q, i), self.dcount[q][i] * 16, sem, q))

    def barrier(self):
        for e in self.ENG:
            self.wait_all_on(e)
        self.last_write = {}
        self.readers = {}

    def close(self):
        for cm in reversed(self._ctx):
            cm.__exit__(None, None, None)


def build_program(stop_after="final", skip_moe=False):
    order = ["attn0", "moe0", "attn1", "moe1", "final"]
    last = order.index(stop_after)
    do = lambda st: order.index(st) <= last
    nc = bass.Bass("TRN2", target_bir_lowering=False)

    def din(name, shape, dt=F32):
        return nc.dram_tensor(name, list(shape), dt, kind="ExternalInput").ap()

    x_in = din("x", [S_LEN, D])
    cT_in = din("cT", [128, KC])
    ada_w = din("ada_w", [2, D, 6 * D])
    ada_b = din("ada_b", [2, 6 * D])
    norm_g = din("norm_g", [2, 2, D])
    na_wqkv = din("na_w_qkv", [D, 3 * D])
    na_wo = din("na_w_o", [D, D])
    na_bias = din("na_bias", [16, 128, NA_TTOT])
    if do("attn1"):
        sw_wqkv = din("sw_w_qkv", [D, 1536])
        sw_wo = din("sw_w_o", [D, D])
        sw_bias = din("sw_bias", [16, 128, 384])
        sw_sinks = din("sw_sinks", [1, 16])
    n_moe = 0 if skip_moe else (2 if do("moe1") else (1 if do("moe0") else 0))
    if n_moe:
        w_router = din("moe_w_router", [n_moe, D, NE])
        w_gate = din("moe_w_gate", [n_moe, NE, D, FF])
        w_up = din("moe_w_up", [n_moe, NE, D, FF])
        w_down = din("moe_w_down", [n_moe, NE, FF, D])
    if do("final"):
        final_g = din("final_g", [1, D])
    out = nc.dram_tensor("out", [S_LEN, D], F32, kind="ExternalOutput").ap()
    xs = nc.dram_tensor("xs_scr", [S_LEN, D], F32, kind="Internal").ap()
    hs = nc.dram_tensor("hs_scr", [S_LEN, D], BF16, kind="Internal").ap()

    S = Sched(nc)
    _uid = [0]

    def sb(name, shape, dt):
        _uid[0] += 1
        return nc.sbuf_tensor("%s_%d" % (name, _uid[0]), shape, dt)

    def ps(name, shape, dt):
        _uid[0] += 1
        return nc.psum_tensor("%s_%d" % (name, _uid[0]), shape, dt)

    with ExitStack() as _st:
        ident = _st.enter_context(sb("ident", [128, 128], BF16))
        identf = _st.enter_context(sb("identf", [128, 128], F32))
        ones = _st.enter_context(sb("ones", [1, 128], F32))
        modt = _st.enter_context(sb("modt", [128, 6, D], F32))
        cbc = _st.enter_context(sb("cbc", [128, KC, 128], F32))
        cact = _st.enter_context(sb("cact", [128, KC], F32))
        rowt = _st.enter_context(sb("rowt", [1, D], F32))
        rowb = _st.enter_context(sb("rowb", [1, 2, 512], F32))
        sinkbc = _st.enter_context(sb("sinkbc", [128, 16], F32))
        S.op("pool", lambda g: g.memset(identf[:], 0.0), writes=["identf"])
        S.op("pool", lambda g: g.affine_select(out=identf[:], in_=identf[:], pattern=[[-1, 128]],
                                               compare_op=ALU.not_equal, fill=1.0, base=0,
                                               channel_multiplier=1),
             reads=["identf"], writes=["identf"])
        S.op("dve", lambda v: v.tensor_copy(out=ident[:], in_=identf[:]), reads=["identf"], writes=["ident"])
        S.op("dve", lambda v: v.memset(ones[:], 1.0), writes=["ones"])
        S.dma("sp", lambda q: q.dma_start(out=cact[:], in_=cT_in), writes=["cact"])
        S.op("act", lambda a: a.activation(out=cact[:], in_=cact[:], func=AF.Silu), reads=["cact"], writes=["cact"])
        S.op("dve", lambda v: v.tensor_copy(out=cbc[:], in_=cact[:].unsqueeze(2).to_broadcast([128, KC, 128])),
             reads=["cact"], writes=["cbc"])
        if do("attn1"):
            S.dma("sp", lambda q: q.dma_start(out=sinkbc[:], in_=sw_sinks.partition_broadcast(128)), writes=["sinkbc"])

        def bcast_row(dst, row_ap, width, pst, pname):
            S.dma("sp", lambda q: q.dma_start(out=rowt[0:1, 0:width], in_=row_ap), writes=["rowt"])
            for j in range(width // 512):
                S.op("pe", lambda pe, j=j: pe.matmul(pst[:, 0:512], lhsT=ones[0:1, :], rhs=rowt[0:1, j * 512:(j + 1) * 512],
                                                     start=True, stop=True),
                     reads=["rowt", "ones"], writes=[pname])
                S.op("act", lambda a, j=j: a.activation(out=dst[:, j * 512:(j + 1) * 512], in_=pst[:, 0:512], func=AF.Copy),
                     reads=[pname], writes=["bc_dst"])

        def mod_phase(l):
            with ExitStack() as _st:
                gbc = _st.enter_context(sb("gbc", [128, 2, D], F32))
                adaw = _st.enter_context(sb("adaw", [128, 2, KC, 512], F32))
                pm = _st.enter_context(ps("pm", [128, 2, 512], F32))
                for i in range(2):
                    bcast_row(gbc[:, i, :], norm_g[l, i:i + 1, :], D, pm[:, 0, :], "pm0")
                wv = ada_w[l].rearrange("(k p) n -> p k n", p=128)
                for cg in range(12):
                    b = cg % 2
                    S.dma("sp", lambda q, cg=cg, b=b: q.dma_start(out=adaw[:, b], in_=wv[:, :, cg * 512:(cg + 1) * 512]),
                          writes=["adaw%d" % b])
                    S.dma("sp", lambda q, cg=cg, b=b: q.dma_start(out=rowb[0:1, b, :], in_=ada_b[l:l + 1, cg * 512:(cg + 1) * 512]),
                          writes=["rowb%d" % b])

                    def f(pe, cg=cg, b=b):
                        for k in range(KC):
                            pe.matmul(pm[:, b, :], lhsT=cbc[:, k, :], rhs=adaw[:, b, k, :], start=(k == 0), stop=False)
                        return pe.matmul(pm[:, b, :], lhsT=ones[0:1, :], rhs=rowb[0:1, b, :],
                                         start=False, stop=True)
                    S.op("pe", f, reads=["adaw%d" % b, "cbc", "rowb%d" % b, "ones"], writes=["pm%d" % b])
                    j, hf = cg // 2, cg % 2
                    dst = modt[:, j, hf * 512:(hf + 1) * 512]
                    if j in (1, 4):
                        gsl = gbc[:, 0 if j == 1 else 1, hf * 512:(hf + 1) * 512]
                        S.op("dve", lambda v, dst=dst, b=b, gsl=gsl: v.scalar_tensor_tensor(
                            out=dst, in0=pm[:, b, :], scalar=1.0, in1=gsl, op0=ALU.add, op1=ALU.mult),
                            reads=["pm%d" % b, "bc_dst"], writes=["modt"])
                    else:
                        S.op("act", lambda a, dst=dst, b=b: a.activation(out=dst, in_=pm[:, b, :], func=AF.Copy),
                             reads=["pm%d" % b], writes=["modt"])
                S.barrier()

        def rms_tile(xt, stat, t, a_ap, sh_ap, out_ap, junk):
            S.op("act", lambda a: a.activation(out=junk, in_=xt, func=AF.Square, accum_out=stat[:, 0:1]),
                 reads=["xt%d" % (t % 2)], writes=["stat", "junk"])
            S.op("dve", lambda v: v.tensor_scalar(out=stat[:, 1:2], in0=stat[:, 0:1], scalar1=1.0 / D, scalar2=EPS,
                                                  op0=ALU.mult, op1=ALU.add), reads=["stat"], writes=["stat"])
            S.op("act", lambda a: a.activation(out=stat[:, 2:3], in_=stat[:, 1:2], func=AF.Sqrt), reads=["stat"], writes=["stat"])
            S.op("dve", lambda v: v.reciprocal(out=stat[:, 3:4], in_=stat[:, 2:3]), reads=["stat"], writes=["stat"])
            S.op("dve", lambda v: v.scalar_tensor_tensor(out=junk, in0=xt, scalar=stat[:, 3:4], in1=a_ap,
                                                         op0=ALU.mult, op1=ALU.mult),
                 reads=["xt%d" % (t % 2), "stat", "modt"], writes=["junk"])

        def attn_phase(l):
            is_na = (l == 0)
            xin = x_in if l == 0 else xs
            wqkv_d = na_wqkv if is_na else sw_wqkv
            wo_d = na_wo if is_na else sw_wo
            bias_w = NA_TTOT if is_na else 384
            with ExitStack() as _st:
                hT = _st.enter_context(sb("hT", [128, KC, S_LEN], BF16))
                attnT = _st.enter_context(sb("attnT", [128, KC, S_LEN], BF16))
                qT = _st.enter_context(sb("qT", [128, S_LEN], BF16))
                kT = _st.enter_context(sb("kT", [128, S_LEN], BF16))
                vv = _st.enter_context(sb("vv", [128, NT, 128], BF16))
                wqkv = _st.enter_context(sb("wqkv", [128, 2, KC, 3, 128], BF16))
                wo = _st.enter_context(sb("wo", [128, KC, D], BF16))
                biast = _st.enter_context(sb("biast", [128, 2, bias_w], F32))
                oall = _st.enter_context(sb("oall", [128, NT, 128], BF16))
                xt = _st.enter_context(sb("xt", [128, 2, D], F32))
                junk = _st.enter_context(sb("junk", [128, D], F32))
                hb = _st.enter_context(sb("hb", [128, 2, D], BF16))
                stat = _st.enter_context(sb("stat", [128, 8], F32))
                ssb = _st.enter_context(sb("ssb", [128, 2, 576], F32))
                pp = _st.enter_context(sb("pp", [128, 2, 576], BF16))
                pTs = _st.enter_context(sb("pTs", [128, 2, 5, 128], BF16))
                st2 = _st.enter_context(sb("st2", [128, 2, 8], F32))
                pS = _st.enter_context(ps("pS", [128, 2, 2, 512], F32))
                pT = _st.enter_context(ps("pT", [128, 2, 8, 128], BF16))
                pV = _st.enter_context(ps("pV", [128, 4, 128], F32))
                pO = _st.enter_context(ps("pO", [128, 2, 64], F32))
                S.dma("pool", lambda g: g.dma_start(out=wo[:], in_=wo_d.rearrange("(k p) n -> p k n", p=128)), writes=["wo"])
                xv = xin.rearrange("(t p) d -> t p d", p=128)
                for t in range(NT):
                    b = t % 2
                    S.dma("sp", lambda q, t=t, b=b: q.dma_start(out=xt[:, b, :], in_=xv[t]), writes=["xt%d" % b])
                    rms_tile(xt[:, b, :], stat, t, modt[:, 1, :], None, None, junk[:])
                    S.op("dve", lambda v, b=b: v.tensor_tensor(out=hb[:, b, :], in0=junk[:], in1=modt[:, 0, :], op=ALU.add),
                         reads=["junk", "modt"], writes=["hb%d" % b])

                    def f(pe, b=b):
                        for k in range(KC):
                            ins = pe.transpose(out=pT[:, b, k, :], in_=hb[:, b, k * 128:(k + 1) * 128], identity=ident[:])
                        return ins
                    S.op("pe", f, reads=["hb%d" % b, "ident"], writes=["pT%d" % b])
                    S.op("act", lambda a, t=t, b=b: a.activation(out=hT[:, :, t * 128:(t + 1) * 128], in_=pT[:, b, :, :], func=AF.Copy),
                         reads=["pT%d" % b], writes=["hT"])
                wsrc = wqkv_d.rearrange("(k p) n -> p k n", p=128)
                for hp in range(8):
                    wb = hp % 2
                    wres = "wqkv%d" % wb
                    if is_na:
                        for j in range(3):
                            S.dma("pool", lambda g, j=j, hp=hp, wb=wb: g.dma_start(
                                out=wqkv[:, wb, :, j, :], in_=wsrc[:, :, j * D + hp * 128: j * D + (hp + 1) * 128]), writes=[wres])
                    else:
                        kvh = hp // 2
                        S.dma("pool", lambda g, hp=hp, wb=wb: g.dma_start(
                            out=wqkv[:, wb, :, 0, :], in_=wsrc[:, :, hp * 128:(hp + 1) * 128]), writes=[wres])
                        for j in (1, 2):
                            c0 = 1024 + (j - 1) * 256 + kvh * 64
                            for hh in range(2):
                                S.dma("pool", lambda g, j=j, hh=hh, wb=wb, c0=c0: g.dma_start(
                                    out=wqkv[:, wb, :, j, hh * 64:(hh + 1) * 64], in_=wsrc[:, :, c0:c0 + 64]), writes=[wres])
                    bsrc = na_bias if is_na else sw_bias
                    for hh in range(2):
                        S.dma("sp", lambda q, hh=hh, hp=hp: q.dma_start(out=biast[:, hh, :], in_=bsrc[2 * hp + hh]),
                              writes=["bias%d" % hh])
                    for j, dstT in ((0, qT), (1, kT)):
                        for tg in range(4):
                            pi = (j * 4 + tg) % 4
                            pst = pS[:, pi // 2, pi % 2, :]

                            def f(pe, j=j, tg=tg, pst=pst, wb=wb):
                                for k in range(KC):
                                    ins = pe.matmul(pst, lhsT=wqkv[:, wb, k, j, :], rhs=hT[:, k, tg * 512:(tg + 1) * 512],
                                                    start=(k == 0), stop=(k == KC - 1))
                                return ins
                            S.op("pe", f, reads=[wres, "hT"], writes=["pS%d" % pi])
                            S.op("act", lambda a, j=j, tg=tg, pst=pst, dstT=dstT: a.activation(
                                out=dstT[:, tg * 512:(tg + 1) * 512], in_=pst, func=AF.Copy, scale=(0.125 if j == 0 else 1.0)),
                                reads=["pS%d" % pi], writes=["qT" if j == 0 else "kT"])
                    for t4 in range(4):
                        def f(pe, t4=t4, wb=wb):
                            for tt in range(4):
                                t = t4 * 4 + tt
                                for k in range(KC):
                                    ins = pe.matmul(pV[:, tt, :], lhsT=hT[:, k, t * 128:(t + 1) * 128], rhs=wqkv[:, wb, k, 2, :],
                                                    start=(k == 0), stop=(k == KC - 1))
                            return ins
                        S.op("pe", f, reads=[wres, "hT"], writes=["pV"])
                        S.op("dve", lambda v, t4=t4: v.tensor_copy(out=vv[:, t4 * 4:(t4 + 1) * 4, :], in_=pV[:]),
                             reads=["pV"], writes=["vv"])
                    it = 0
                    for hh in range(2):
                        h = 2 * hp + hh
                        hsl = slice(hh * 64, (hh + 1) * 64)
                        for qb in range(NT):
                            if is_na:
                                r0 = 2 * qb
                                if r0 <= 2:
                                    krow, tt_ = 0, r0 // 2
                                elif r0 >= 28:
                                    krow, tt_ = 24, 3 + (r0 - 28) // 2
                                else:
                                    krow, tt_ = r0 - 4, 2
                                nk = NA_TW[tt_]
                                boff = NA_TOFF[tt_]
                                ks = krow * 64
                            else:
                                t_lo, t_hi = max(qb - 1, 0), min(qb + 1, NT - 1)
                                nk = (t_hi - t_lo + 1) * 128
                                ks = t_lo * 128
                                boff = 128 if qb == 0 else 0
                            half = nk // 2
                            sb_ = it % 2
                            it += 1
                            kt0 = ks // 128
                            nfull = nk // 128
                            rem = nk - nfull * 128

                            def f(pe, qb=qb, ks=ks, half=half, sb_=sb_, hsl=hsl):
                                for pc in range(2):
                                    ins = pe.matmul(pS[:, sb_, pc, 0:half], lhsT=qT[hsl, qb * 128:(qb + 1) * 128],
                                                    rhs=kT[hsl, ks + pc * half: ks + (pc + 1) * half], start=True, stop=True)
                                return ins
                            S.op("pe", f, reads=["qT", "kT"], writes=["pS%d" % (2 * sb_), "pS%d" % (2 * sb_ + 1)])
                            s_v = ssb[:, sb_, 0:nk]
                            S.op("dve", lambda v, sb_=sb_, half=half, nk=nk, boff=boff, hh=hh: v.tensor_tensor(
                                out=ssb[:, sb_, 0:nk].rearrange("p (c n) -> p c n", c=2), in0=pS[:, sb_, :, 0:half],
                                in1=biast[:, hh, boff:boff + nk].rearrange("p (c n) -> p c n", c=2), op=ALU.add),
                                reads=["pS%d" % (2 * sb_), "pS%d" % (2 * sb_ + 1), "bias%d" % hh], writes=["ssb%d" % sb_])
                            stt = st2[:, sb_, :]
                            if is_na:
                                S.op("dve", lambda v, s_v=s_v, stt=stt: v.tensor_reduce(out=stt[:, 0:1], in_=s_v, axis=AX.X, op=ALU.max, negate=True),
                                     reads=["ssb%d" % sb_], writes=["st2_%d" % sb_])
                            else:
                                S.op("dve", lambda v, s_v=s_v, stt=stt: v.tensor_reduce(out=stt[:, 4:5], in_=s_v, axis=AX.X, op=ALU.max),
                                     reads=["ssb%d" % sb_], writes=["st2_%d" % sb_])
                                S.op("dve", lambda v, stt=stt, h=h: v.tensor_scalar(out=stt[:, 0:1], in0=stt[:, 4:5], scalar1=sinkbc[:, h:h + 1],
                                                                                     scalar2=-1.0, op0=ALU.max, op1=ALU.mult),
                                     reads=["st2_%d" % sb_, "sinkbc"], writes=["st2_%d" % sb_])
                            S.op("act", lambda a, s_v=s_v, stt=stt, sb_=sb_, nk=nk: a.activation(
                                out=pp[:, sb_, 0:nk], in_=s_v, func=AF.Exp, bias=stt[:, 0:1], scale=1.0, accum_out=stt[:, 1:2]),
                                reads=["ssb%d" % sb_, "st2_%d" % sb_], writes=["pp%d" % sb_, "st2_%d" % sb_])
                            if not is_na:
                                S.op("act", lambda a, stt=stt, h=h: a.activation(out=stt[:, 5:6], in_=sinkbc[:, h:h + 1], func=AF.Exp,
                                                                                 bias=stt[:, 0:1], scale=1.0),
                                     reads=["st2_%d" % sb_, "sinkbc"], writes=["st2_%d" % sb_])
                                S.op("dve", lambda v, stt=stt: v.tensor_tensor(out=stt[:, 1:2], in0=stt[:, 1:2], in1=stt[:, 5:6], op=ALU.add),
                                     reads=["st2_%d" % sb_], writes=["st2_%d" % sb_])
                            S.op("dve", lambda v, stt=stt: v.reciprocal(out=stt[:, 2:3], in_=stt[:, 1:2]),
                                 reads=["st2_%d" % sb_], writes=["st2_%d" % sb_])

                            def f(pe, sb_=sb_, nfull=nfull, rem=rem):
                                for c in range(nfull):
                                    ins = pe.transpose(out=pT[:, sb_, c, :], in_=pp[:, sb_, c * 128:(c + 1) * 128], identity=ident[:])
                                if rem:
                                    ins = pe.transpose(out=pT[0:rem, sb_, nfull, :], in_=pp[:, sb_, nfull * 128: nfull * 128 + rem],
                                                       identity=ident[:])
                                return ins
                            S.op("pe", f, reads=["pp%d" % sb_, "ident"], writes=["pT%d" % sb_])
                            S.op("act", lambda a, sb_=sb_, nfull=nfull: a.activation(out=pTs[:, sb_, 0:nfull, :], in_=pT[:, sb_, 0:nfull, :], func=AF.Copy),
                                 reads=["pT%d" % sb_], writes=["pTs%d" % sb_])
                            if rem:
                                S.op("act", lambda a, sb_=sb_, nfull=nfull, rem=rem: a.activation(
                                    out=pTs[0:rem, sb_, nfull, :], in_=pT[0:rem, sb_, nfull, :], func=AF.Copy),
                                    reads=["pT%d" % sb_], writes=["pTs%d" % sb_])

                            def f(pe, sb_=sb_, nfull=nfull, rem=rem, kt0=kt0, hsl=hsl):
                                tot = nfull + (1 if rem else 0)
                                for c in range(nfull):
                                    ins = pe.matmul(pO[:, sb_, :], lhsT=pTs[:, sb_, c, :], rhs=vv[:, kt0 + c, hsl],
                                                    start=(c == 0), stop=(c == tot - 1))
                                if rem:
                                    ins = pe.matmul(pO[:, sb_, :], lhsT=pTs[0:rem, sb_, nfull, :], rhs=vv[0:rem, kt0 + nfull, hsl],
                                                    start=False, stop=True)
                                return ins
                            S.op("pe", f, reads=["pTs%d" % sb_, "vv"], writes=["pO%d" % sb_])
                            S.op("act", lambda a, sb_=sb_, qb=qb, hsl=hsl, stt=stt: a.activation(
                                out=oall[:, qb, hsl], in_=pO[:, sb_, :], func=AF.Copy, scale=stt[:, 2:3]),
                                reads=["pO%d" % sb_, "st2_%d" % sb_], writes=["oall"])
                    for t4 in range(4):
                        b = t4 % 2

                        def f(pe, t4=t4, b=b):
                            for tt in range(4):
                                ins = pe.transpose(out=pT[:, b, tt, :], in_=oall[:, t4 * 4 + tt, :], identity=ident[:])
                            return ins
                        S.op("pe", f, reads=["oall", "ident"], writes=["pT%d" % b])
                        S.op("dve", lambda v, t4=t4, b=b, hp=hp: v.tensor_copy(
                            out=attnT[:, hp, t4 * 512:(t4 + 1) * 512].rearrange("p (t n) -> p t n", t=4), in_=pT[:, b, 0:4, :]),
                            reads=["pT%d" % b], writes=["attnT"])
                xo = xs.rearrange("(t p) d -> t p d", p=128)
                for t in range(NT):
                    b = t % 2
                    S.dma("sp", lambda q, t=t, b=b: q.dma_start(out=xt[:, b, :], in_=xv[t]), writes=["xt%d" % b])

                    def f(pe, t=t, b=b):
                        for n in range(2):
                            for k in range(KC):
                                ins = pe.matmul(pS[:, b, n, :], lhsT=attnT[:, k, t * 128:(t + 1) * 128], rhs=wo[:, k, n * 512:(n + 1) * 512],
                                                start=(k == 0), stop=(k == KC - 1))
                        return ins
                    S.op("pe", f, reads=["attnT", "wo"], writes=["pS%d" % (2 * b), "pS%d" % (2 * b + 1)])
                    S.op("dve", lambda v, b=b: v.tensor_tensor(out=junk[:].rearrange("p (n c) -> p n c", n=2), in0=pS[:, b, :, :],
                                                                in1=modt[:, 2, :].rearrange("p (n c) -> p n c", n=2), op=ALU.mult),
                         reads=["pS%d" % (2 * b), "pS%d" % (2 * b + 1), "modt"], writes=["junk"])
                    S.op("dve", lambda v, b=b: v.tensor_tensor(out=xt[:, b, :], in0=xt[:, b, :], in1=junk[:], op=ALU.add),
                         reads=["junk", "xt%d" % b], writes=["xt%d" % b])
                    S.dma("sp", lambda q, t=t, b=b: q.dma_start(out=xo[t], in_=xt[:, b, :]), reads=["xt%d" % b], writes=["xs"])
                S.barrier()

        def moe_phase(l):
            RING = 12
            with sb("ring", [128, RING, 4096], BF16) as ring:
                chunk_no = [0]

                def load_chunk(kind, e, i):
                    slot = chunk_no[0] % RING
                    chunk_no[0] += 1
                    res = "ring%d" % slot
                    if kind == "down":
                        src = w_down[l, e, i * 512:(i + 1) * 512, :].rearrange("(j p) n -> p j n", p=128)
                        dst = ring[:, slot, :].rearrange("p (j n) -> p j n", j=4)
                    else:
                        wsrc = w_gate if kind == "gate" else w_up
                        src = wsrc[l, e, :, i * 512:(i + 1) * 512].rearrange("(k p) n -> p k n", p=128)
                        dst = ring[:, slot, :].rearrange("p (k n) -> p k n", k=KC)
                    S.dma("pool", lambda g: g.dma_start(out=dst, in_=src), writes=[res])
                    return slot

                def load_expert(e):
                    sl = {}
                    for fg in range(4):
                        sl[("gate", fg)] = load_chunk("gate", e, fg)
                        sl[("up", fg)] = load_chunk("up", e, fg)
                    for i in range(4):
                        sl[("down", i)] = load_chunk("down", e, i)
                    return sl

                slots0 = load_expert(0)
                with ExitStack() as _st:
                    idxT = _st.enter_context(sb("idxT", [128, 2, NE], U32))
                    valsT = _st.enter_context(sb("valsT", [128, 2, NE], F32))
                    with ExitStack() as _st:
                        xt = _st.enter_context(sb("xt", [128, 2, D], F32))
                        junk = _st.enter_context(sb("junk", [128, D], F32))
                        h2f = _st.enter_context(sb("h2f", [128, 2, D], F32))
                        h2b = _st.enter_context(sb("h2b", [128, 2, D], BF16))
                        h2T = _st.enter_context(sb("h2T", [128, 2, KC, 128], F32))
                        wr = _st.enter_context(sb("wr", [128, KC, NE], F32))
                        stat = _st.enter_context(sb("stat", [128, 8], F32))
                        lg = _st.enter_context(sb("lg", [128, 2, NE], F32))
                        cur = _st.enter_context(sb("cur", [16, 2, S_LEN], F32))
                        vals = _st.enter_context(sb("vals", [16, CAP], F32))
                        idx = _st.enter_context(sb("idx", [16, CAP], U32))
                        idxf = _st.enter_context(sb("idxf", [16, CAP], F32))
                        pF = _st.enter_context(ps("pF", [128, 2, 2, 4, 128], F32))
                        pL = _st.enter_context(ps("pL", [128, NE], F32))
                        pA = _st.enter_context(ps("pA", [16, 128], F32))
                        pI = _st.enter_context(ps("pI", [128, 2, NE], F32))
                        S.dma("sp", lambda q: q.dma_start(out=wr[:], in_=w_router[l].rearrange("(k p) n -> p k n", p=128)), writes=["wr"])
                        xv = xs.rearrange("(t p) d -> t p d", p=128)
                        hv = hs.rearrange("(t p) d -> t p d", p=128)
                        for t in range(NT):
                            b = t % 2
                            S.dma("sp", lambda q, t=t, b=b: q.dma_start(out=xt[:, b, :], in_=xv[t]), reads=["xs"], writes=["xt%d" % b])
                            rms_tile(xt[:, b, :], stat, t, modt[:, 4, :], None, None, junk[:])
                            S.op("dve", lambda v, b=b: v.tensor_tensor(out=h2f[:, b, :], in0=junk[:], in1=modt[:, 3, :], op=ALU.add),
                                 reads=["junk", "modt"], writes=["h2f%d" % b])
                            S.op("act", lambda a, b=b: a.activation(out=h2b[:, b, :], in_=h2f[:, b, :], func=AF.Copy),
                                 reads=["h2f%d" % b], writes=["h2b%d" % b])
                            S.dma("sp", lambda q, t=t, b=b: q.dma_start(out=hv[t], in_=h2b[:, b, :]), reads=["h2b%d" % b], writes=["hs"])
                            for g2 in range(2):
                                def f(pe, b=b, g2=g2):
                                    for kk in range(4):
                                        k = g2 * 4 + kk
                                        ins = pe.transpose(out=pF[:, b, g2, kk, :], in_=h2f[:, b, k * 128:(k + 1) * 128], identity=identf[:])
                                    return ins
                                S.op("pe", f, reads=["h2f%d" % b, "identf"], writes=["pF%d_%d" % (b, g2)])
                                S.op("act", lambda a, b=b, g2=g2: a.activation(out=h2T[:, b, g2 * 4:(g2 + 1) * 4, :], in_=pF[:, b, g2, :, :], func=AF.Copy),
                                     reads=["pF%d_%d" % (b, g2)], writes=["h2T%d" % b])

                            def f(pe, b=b):
                                for k in range(KC):
                                    ins = pe.matmul(pL[:], lhsT=h2T[:, b, k, :], rhs=wr[:, k, :], start=(k == 0), stop=(k == KC - 1))
                                return ins
                            S.op("pe", f, reads=["h2T%d" % b, "wr"], writes=["pL"])
                            S.op("dve", lambda v: v.tensor_reduce(out=stat[:, 4:5], in_=pL[:], axis=AX.X, op=ALU.max, negate=True),
                                 reads=["pL"], writes=["stat2"])
                            S.op("act", lambda a, b=b: a.activation(out=lg[:, b, :], in_=pL[:], func=AF.Exp, bias=stat[:, 4:5], scale=1.0,
                                                                    accum_out=stat[:, 5:6]),
                                 reads=["pL", "stat2"], writes=["lg%d" % b, "stat2"])
                            S.op("dve", lambda v: v.reciprocal(out=stat[:, 6:7], in_=stat[:, 5:6]), reads=["stat2"], writes=["stat2"])
                            S.op("dve", lambda v, b=b: v.tensor_scalar(out=lg[:, b, :], in0=lg[:, b, :], scalar1=stat[:, 6:7], scalar2=None,
                                                                       op0=ALU.mult), reads=["lg%d" % b, "stat2"], writes=["lg%d" % b])
                            S.op("pe", lambda pe, b=b: pe.transpose(out=pA[:], in_=lg[:, b, :], identity=identf[:]),
                                 reads=["lg%d" % b, "identf"], writes=["pA"])
                            S.op("act", lambda a, t=t: a.activation(out=cur[:, 0, t * 128:(t + 1) * 128], in_=pA[:], func=AF.Copy),
                                 reads=["pA"], writes=["cur0"])
                        for r in range(CAP // 8):
                            c, n = r % 2, (r + 1) % 2
                            rs_ = slice(r * 8, (r + 1) * 8)
                            S.op("dve", lambda v, rs_=rs_, c=c: v.max(out=vals[:, rs_], in_=cur[:, c, :]), reads=["cur%d" % c], writes=["vals"])
                            S.op("dve", lambda v, rs_=rs_, c=c: v.max_index(out=idx[:, rs_], in_max=vals[:, rs_], in_values=cur[:, c, :]),
                                 reads=["cur%d" % c, "vals"], writes=["idx"])
                            S.op("dve", lambda v, rs_=rs_, c=c, n=n: v.match_replace(out=cur[:, n, :], in_to_replace=vals[:, rs_],
                                                                                    in_values=cur[:, c, :], imm_value=-1.0),
                                 reads=["cur%d" % c, "vals"], writes=["cur%d" % n])
                        S.op("dve", lambda v: v.tensor_copy(out=idxf[:], in_=idx[:]), reads=["idx"], writes=["idxf"])
                        for src_t, dst_t, nm in ((idxf, idxT, "idxT"), (vals, valsT, "valsT")):
                            def f(pe, src_t=src_t):
                                for hh in range(2):
                                    ins = pe.transpose(out=pI[:, hh, :], in_=src_t[:, hh * 128:(hh + 1) * 128], identity=identf[0:16, 0:16])
                                return ins
                            S.op("pe", f, reads=["idxf", "vals", "identf"], writes=["pI"])
                            S.op("dve", lambda v, dst_t=dst_t: v.tensor_copy(out=dst_t[:], in_=pI[:]), reads=["pI"], writes=[nm])
                        S.barrier()
                    with ExitStack() as _st:
                        xg = _st.enter_context(sb("xg", [128, 2, 2, D], BF16))
                        xgT = _st.enter_context(sb("xgT", [128, 2, KC, CAP], BF16))
                        actT = _st.enter_context(sb("actT", [128, 2, 16, CAP], BF16))
                        sg = _st.enter_context(sb("sg", [128, 2, CAP], F32))
                        ys = _st.enter_context(sb("ys", [128, 2, D], F32))
                        pX = _st.enter_context(ps("pX", [128, 2, KC, 128], BF16))
                        pG = _st.enter_context(ps("pG", [128, 2, 2, CAP], F32))
                        pY = _st.enter_context(ps("pY", [128, 2, 2, 512], F32))
                        slots = slots0

                        def gather(e):
                            eb = e % 2
                            for hf in range(2):
                                S.dma("pool", lambda g, e=e, hf=hf, eb=eb: g.indirect_dma_start(
                                    out=xg[:, eb, hf, :], out_offset=None, in_=hs,
                                    in_offset=bass.IndirectOffsetOnAxis(ap=idxT[:, hf, e:e + 1], axis=0)),
                                    reads=["idxT", "hs"], writes=["xg%d_%d" % (eb, hf)])
                        gather(0)
                        for e in range(NE):
                            eb = e % 2
                            for hf in range(2):
                                def f(pe, eb=eb, hf=hf):
                                    for k in range(KC):
                                        ins = pe.transpose(out=pX[:, hf, k, :], in_=xg[:, eb, hf, k * 128:(k + 1) * 128], identity=ident[:])
                                    return ins
                                S.op("pe", f, reads=["xg%d_%d" % (eb, hf), "ident"], writes=["pX%d" % hf])
                                S.op("act", lambda a, eb=eb, hf=hf: a.activation(out=xgT[:, eb, :, hf * 128:(hf + 1) * 128], in_=pX[:, hf, :, :], func=AF.Copy),
                                     reads=["pX%d" % hf], writes=["xgT%d" % eb])
                            fi_n = 0
                            for fg in range(4):
                                sg_ = slots[("gate", fg)]
                                su_ = slots[("up", fg)]
                                gch = ring[:, sg_, :].rearrange("p (k n) -> p k n", k=KC)
                                uch = ring[:, su_, :].rearrange("p (k n) -> p k n", k=KC)
                                for fs in range(4):
                                    fi = fg * 4 + fs
                                    pb = fi % 2

                                    def f(pe, gch=gch, uch=uch, fs=fs, pb=pb, eb=eb):
                                        for j, ch in ((0, gch), (1, uch)):
                                            for k in range(KC):
                                                ins = pe.matmul(pG[:, pb, j, :], lhsT=ch[:, k, fs * 128:(fs + 1) * 128], rhs=xgT[:, eb, k, :],
                                                                start=(k == 0), stop=(k == KC - 1))
                                        return ins
                                    S.op("pe", f, reads=["ring%d" % sg_, "ring%d" % su_, "xgT%d" % eb], writes=["pG%d" % pb])
                                    S.op("act", lambda a, pb=pb: a.activation(out=sg[:, pb, :], in_=pG[:, pb, 0, :], func=AF.Silu),
                                         reads=["pG%d" % pb], writes=["sg%d" % pb])
                                    S.op("dve", lambda v, pb=pb, fi=fi, eb=eb: v.tensor_tensor(out=actT[:, eb, fi, :], in0=sg[:, pb, :], in1=pG[:, pb, 1, :],
                                                                                              op=ALU.mult),
                                         reads=["sg%d" % pb, "pG%d" % pb], writes=["actT%d" % eb])
                            for tt in range(2):
                                def f(pe, tt=tt, eb=eb, slots=slots):
                                    for n in range(2):
                                        for fi in range(16):
                                            dch = ring[:, slots[("down", fi // 4)], :].rearrange("p (j n) -> p j n", j=4)
                                            ins = pe.matmul(pY[:, tt, n, :], lhsT=actT[:, eb, fi, tt * 128:(tt + 1) * 128],
                                                            rhs=dch[:, fi % 4, n * 512:(n + 1) * 512], start=(fi == 0), stop=(fi == 15))
                                    return ins
                                S.op("pe", f, reads=["actT%d" % eb] + ["ring%d" % slots[("down", i)] for i in range(4)],
                                     writes=["pY%d" % tt])
                                S.op("dve", lambda v, tt=tt, e=e: v.scalar_tensor_tensor(
                                    out=ys[:, tt, :].rearrange("p (n c) -> p n c", n=2), in0=pY[:, tt, :, :], scalar=valsT[:, tt, e:e + 1],
                                    in1=modt[:, 5, :].rearrange("p (n c) -> p n c", n=2), op0=ALU.mult, op1=ALU.mult),
                                    reads=["pY%d" % tt, "valsT", "modt"], writes=["ys%d" % tt])
                            if e + 1 < NE:
                                slots_next = load_expert(e + 1)
                                gather(e + 1)
                            for tt in range(2):
                                S.dma("pool", lambda g, tt=tt, e=e: g.indirect_dma_start(
                                    out=xs, out_offset=bass.IndirectOffsetOnAxis(ap=idxT[:, tt, e:e + 1], axis=0),
                                    in_=ys[:, tt, :], in_offset=None, compute_op=ALU.add),
                                    reads=["ys%d" % tt, "idxT", "xs"], writes=["xs"])
                            if e + 1 < NE:
                                slots = slots_next
                        S.barrier()

        def final_phase():
            with ExitStack() as _st:
                fgb = _st.enter_context(sb("fgb", [128, D], F32))
                xt = _st.enter_context(sb("xt", [128, 2, D], F32))
                junk = _st.enter_context(sb("junk", [128, D], F32))
                ot = _st.enter_context(sb("ot", [128, 2, D], F32))
                stat = _st.enter_context(sb("stat", [128, 8], F32))
                pm = _st.enter_context(ps("pm", [128, 512], F32))
                bcast_row(fgb[:], final_g, D, pm[:], "pmf")
                xv = xs.rearrange("(t p) d -> t p d", p=128)
                ov = out.rearrange("(t p) d -> t p d", p=128)
                for t in range(NT):
                    b = t % 2
                    S.dma("sp", lambda q, t=t, b=b: q.dma_start(out=xt[:, b, :], in_=xv[t]), reads=["xs"], writes=["xt%d" % b])
                    S.op("act", lambda a, b=b: a.activation(out=junk[:], in_=xt[:, b, :], func=AF.Square, accum_out=stat[:, 0:1]),
                         reads=["xt%d" % b], writes=["stat", "junk"])
                    S.op("dve", lambda v: v.tensor_scalar(out=stat[:, 1:2], in0=stat[:, 0:1], scalar1=1.0 / D, scalar2=EPS,
                                                          op0=ALU.mult, op1=ALU.add), reads=["stat"], writes=["stat"])
                    S.op("act", lambda a: a.activation(out=stat[:, 2:3], in_=stat[:, 1:2], func=AF.Sqrt), reads=["stat"], writes=["stat"])
                    S.op("dve", lambda v: v.reciprocal(out=stat[:, 3:4], in_=stat[:, 2:3]), reads=["stat"], writes=["stat"])
                    S.op("dve", lambda v, b=b: v.scalar_tensor_tensor(out=ot[:, b, :], in0=xt[:, b, :], scalar=stat[:, 3:4], in1=fgb[:],
                                                                      op0=ALU.mult, op1=ALU.mult),
                         reads=["xt%d" % b, "stat", "bc_dst"], writes=["ot%d" % b])
                    S.dma("sp", lambda q, t=t, b=b: q.dma_start(out=ov[t], in_=ot[:, b, :]), reads=["ot%d" % b], writes=["out"])

        def copy_out():
            with sb("xt", [128, 2, D], F32) as xt:
                xv = xs.rearrange("(t p) d -> t p d", p=128)
                ov = out.rearrange("(t p) d -> t p d", p=128)
                for t in range(NT):
                    b = t % 2
                    S.dma("sp", lambda q, t=t, b=b: q.dma_start(out=xt[:, b, :], in_=xv[t]), reads=["xs"], writes=["xt%d" % b])
                    S.dma("sp", lambda q, t=t, b=b: q.dma_start(out=ov[t], in_=xt[:, b, :]), reads=["xt%d" % b], writes=["out"])

        for l in range(2):
            if not do("attn%d" % l):
                break
            mod_phase(l)
            attn_phase(l)
            if do("moe%d" % l) and not skip_moe:
                moe_phase(l)
        if do("final"):
            final_phase()
        else:
            copy_out()
        for e in Sched.ENG:
            S.wait_all_on(e)
    S.close()
    return nc, S


def _t5_buckets(rel):
    half, max_exact = 16, 8
    n = np.abs(rel)
    large = max_exact + (np.log(np.maximum(n, 1) / max_exact) / np.log(128 / max_exact) * (half - max_exact)).astype(np.int32)
    large = np.minimum(large, half - 1)
    return (rel > 0).astype(np.int32) * half + np.where(n < max_exact, n, large)


def _na_bias_tables(rpb):
    out = np.full((16, 128, NA_TTOT), NEG, np.float32)
    p = np.arange(128)
    rq, cq = p // 64, p % 64
    for tt_, r0 in enumerate([0, 2, 10, 28, 30]):
        if r0 <= 2:
            krow0 = 0
        elif r0 >= 28:
            krow0 = 24
        else:
            krow0 = r0 - 4
        nk = NA_TW[tt_]
        j = np.arange(nk)
        krow = krow0 + j // 64
        kcol = j % 64
        r = r0 + rq
        rs = np.clip(r - 4, 0, 24)
        cstart = np.clip(cq - 8, 0, 48)
        ok = ((krow[None, :] >= rs[:, None]) & (krow[None, :] < rs[:, None] + 8)
              & (kcol[None, :] >= cstart[:, None]) & (kcol[None, :] < cstart[:, None] + 16))
        dr = np.clip(krow[None, :] - r[:, None] + 7, 0, 14)
        dc = np.clip(kcol[None, :] - cq[:, None] + 15, 0, 30)
        vals = rpb[:, dr, dc]
        blk = np.where(ok[None], vals, np.float32(NEG)).astype(np.float32)
        out[:, :, NA_TOFF[tt_]:NA_TOFF[tt_] + nk] = blk
    return out


def _sw_bias_table(t5):
    rel = (np.arange(384)[None, :] - 128) - np.arange(128)[:, None]
    b = np.transpose(t5[_t5_buckets(rel)], (2, 0, 1))
    return np.where((np.abs(rel) <= 128)[None], b, np.float32(NEG)).astype(np.float32)


def make_in_maps(inputs, cores, stop_after="final", skip_moe=False):
    order = ["attn0", "moe0", "attn1", "moe1", "final"]
    do = lambda st: order.index(st) <= order.index(stop_after)
    f32 = lambda a: np.ascontiguousarray(np.asarray(a, dtype=np.float32))
    shared = {
        "ada_w": f32(inputs["ada_w"]), "ada_b": f32(inputs["ada_b"]), "norm_g": f32(inputs["norm_g"]),
        "na_w_qkv": f32(inputs["na_w_qkv"][0]), "na_w_o": f32(inputs["na_w_o"][0]),
        "na_bias": _na_bias_tables(f32(inputs["na_rpb"][0])),
    }
    if do("attn1"):
        shared.update({"sw_w_qkv": f32(inputs["sw_w_qkv"][0]), "sw_w_o": f32(inputs["sw_w_o"][0]),
                       "sw_bias": _sw_bias_table(f32(inputs["t5_bias"])), "sw_sinks": f32(inputs["sw_sinks"]).reshape(1, 16)})
    n_moe = 0 if skip_moe else (2 if do("moe1") else (1 if do("moe0") else 0))
    if n_moe:
        shared.update({"moe_w_router": f32(inputs["moe_w_router"][:n_moe]), "moe_w_gate": f32(inputs["moe_w_gate"][:n_moe]),
                       "moe_w_up": f32(inputs["moe_w_up"][:n_moe]), "moe_w_down": f32(inputs["moe_w_down"][:n_moe])})
    if do("final"):
        shared["final_g"] = f32(inputs["final_g"]).reshape(1, D)
    x = f32(inputs["x"])
    c = f32(inputs["c"])
    maps = []
    for b in cores:
        m = dict(shared)
        m["x"] = np.ascontiguousarray(x[b])
        m["cT"] = np.ascontiguousarray(c[b].reshape(KC, 128).T)
        maps.append(m)
    return maps


_PROG = {}


def kernel(**inputs):
    if "final" not in _PROG:
        _PROG["final"] = build_program("final")[0]
    nc = _PROG["final"]
    cores = list(range(8))
    in_maps = make_in_maps(inputs, cores)
    res = run_bass_kernel_spmd(nc, in_maps, core_ids=cores)
    return np.stack([np.asarray(r["out"], dtype=np.float32) for r in res.results], axis=0)
```

```python
import numpy as np
from contextlib import ExitStack
import concourse.bass as bass
import concourse.mybir as mybir
from concourse.bass_utils import run_bass_kernel_spmd

F32 = mybir.dt.float32
BF16 = mybir.dt.bfloat16
U32 = mybir.dt.uint32
AF = mybir.ActivationFunctionType
ALU = mybir.AluOpType
AX = mybir.AxisListType

S_LEN = 2048
D = 1024
NT = 16
KC = 8
NE = 16
FF = 2048
CAP = 256
EPS = 1e-6
NEG = -1e30
NA_TW = [512, 512, 576, 512, 512]
NA_TOFF = [0, 512, 1024, 1600, 2112]
NA_TTOT = 2624


class Sched:
    ENG = ("pe", "act", "dve", "pool", "sp")

    def __init__(self, nc, n_dma_sems=8):
        self.nc = nc
        self.eng = {"pe": nc.tensor, "act": nc.scalar, "dve": nc.vector,
                    "pool": nc.gpsimd, "sp": nc.sync}
        self._ctx = []
        self.csem = {}
        self.ccount = {e: 0 for e in self.ENG}
        for e in self.ENG:
            self.csem[e] = self._sem("c_" + e)
        self.dsem = {}
        self.dcount = {}
        self.dnext = {}
        for q in ("sp", "act", "pool"):
            self.dsem[q] = [self._sem("d_%s%d" % (q, i)) for i in range(n_dma_sems)]
            self.dcount[q] = [0] * n_dma_sems
            self.dnext[q] = 0
        self.last_write = {}
        self.readers = {}
        self.seen = {e: {} for e in self.ENG}
        self.n_wait = 0
        self.n_ops = 0

    def _sem(self, name):
        cm = self.nc.semaphore(name)
        s = cm.__enter__()
        self._ctx.append(cm)
        return s

    def _need(self, e, tok):
        kind, key, val, sem, src = tok
        if kind == "c" and src == e and e == "pe":
            return
        k = (kind, key)
        if self.seen[e].get(k, 0) >= val:
            return
        self.seen[e][k] = val
        self.eng[e].wait_ge(sem, val)
        self.n_wait += 1

    def _deps(self, e, reads, writes):
        toks = []
        for r in reads:
            t = self.last_write.get(r)
            if t is not None:
                toks.append(t)
        for w in writes:
            t = self.last_write.get(w)
            if t is not None:
                toks.append(t)
            toks.extend(self.readers.get(w, ()))
        for t in toks:
            self._need(e, t)

    def _commit(self, tok, reads, writes):
        for r in reads:
            self.readers.setdefault(r, []).append(tok)
        for w in writes:
            self.last_write[w] = tok
            self.readers[w] = []

    def op(self, e, fn, reads=(), writes=()):
        self._deps(e, reads, writes)
        ins = fn(self.eng[e])
        self.ccount[e] += 1
        ins.then_inc(self.csem[e], 1)
        tok = ("c", e, self.ccount[e], self.csem[e], e)
        self._commit(tok, reads, writes)
        self.n_ops += 1
        return tok

    def dma(self, q, fn, reads=(), writes=()):
        i = self.dnext[q]
        self.dnext[q] = (i + 1) % len(self.dsem[q])
        sem = self.dsem[q][i]
        if self.dcount[q][i] > 0:
            self._need(q, ("d", (q, i), self.dcount[q][i] * 16, sem, q))
        self._deps(q, reads, writes)
        ins = fn(self.eng[q])
        self.dcount[q][i] += 1
        ins.then_inc(sem, 16)
        tok = ("d", (q, i), self.dcount[q][i] * 16, sem, q)
        self._commit(tok, reads, writes)
        self.n_ops += 1
        return tok

    def wait_all_on(self, e):
        for f in self.ENG:
            if self.ccount[f] > 0:
                self._need(e, ("c", f, self.ccount[f], self.csem[f], f))
        for q in self.dsem:
            for i, sem in enumerate(self.dsem[q]):
                if self.dcount[q][i] > 0:
                    self._need(e, ("d", (q, i), self.dcount[q][i] * 16, sem, q))

    def barrier(self):
        for e in self.ENG:
            self.wait_all_on(e)
        self.last_write = {}
        self.readers = {}

    def close(self):
        for cm in reversed(self._ctx):
            cm.__exit__(None, None, None)


def build_program(stop_after="final", skip_moe=False):
    order = ["attn0", "moe0", "attn1", "moe1", "final"]
    last = order.index(stop_after)
    do = lambda st: order.index(st) <= last
    nc = bass.Bass("TRN2", target_bir_lowering=False)

    def din(name, shape, dt=F32):
        return nc.dram_tensor(name, list(shape), dt, kind="ExternalInput").ap()

    x_in = din("x", [S_LEN, D])
    cT_in = din("cT", [128, KC])
    ada_w = din("ada_w", [2, D, 6 * D])
    ada_b = din("ada_b", [2, 6 * D])
    norm_g = din("norm_g", [2, 2, D])
    na_wqkv = din("na_w_qkv", [D, 3 * D])
    na_wo = din("na_w_o", [D, D])
    na_bias = din("na_bias", [16, 128, NA_TTOT])
    if do("attn1"):
        sw_wqkv = din("sw_w_qkv", [D, 1536])
        sw_wo = din("sw_w_o", [D, D])
        sw_bias = din("sw_bias", [16, 128, 384])
        sw_sinks = din("sw_sinks", [1, 16])
    n_moe = 0 if skip_moe else (2 if do("moe1") else (1 if do("moe0") else 0))
    if n_moe:
        w_router = din("moe_w_router", [n_moe, D, NE])
        w_gate = din("moe_w_gate", [n_moe, NE, D, FF])
        w_up = din("moe_w_up", [n_moe, NE, D, FF])
        w_down = din("moe_w_down", [n_moe, NE, FF, D])
    if do("final"):
        final_g = din("final_g", [1, D])
    out = nc.dram_tensor("out", [S_LEN, D], F32, kind="ExternalOutput").ap()
    xs = nc.dram_tensor("xs_scr", [S_LEN, D], F32, kind="Internal").ap()
    hs = nc.dram_tensor("hs_scr", [S_LEN, D], BF16, kind="Internal").ap()

    S = Sched(nc)
    _uid = [0]

    def sb(name, shape, dt):
        _uid[0] += 1
        return nc.sbuf_tensor("%s_%d" % (name, _uid[0]), shape, dt)

    def ps(name, shape, dt):
        _uid[0] += 1
        return nc.psum_tensor("%s_%d" % (name, _uid[0]), shape, dt)

    with ExitStack() as _st:
        ident = _st.enter_context(sb("ident", [128, 128], BF16))
        identf = _st.enter_context(sb("identf", [128, 128], F32))
        ones = _st.enter_context(sb("ones", [1, 128], F32))
        modt = _st.enter_context(sb("modt", [128, 6, D], F32))
        cbc = _st.enter_context(sb("cbc", [128, KC, 128], F32))
        cact = _st.enter_context(sb("cact", [128, KC], F32))
        rowt = _st.enter_context(sb("rowt", [1, D], F32))
        rowb = _st.enter_context(sb("rowb", [1, 2, 512], F32))
        sinkbc = _st.enter_context(sb("sinkbc", [128, 16], F32))
        S.op("pool", lambda g: g.memset(identf[:], 0.0), writes=["identf"])
        S.op("pool", lambda g: g.affine_select(out=identf[:], in_=identf[:], pattern=[[-1, 128]],
                                               compare_op=ALU.not_equal, fill=1.0, base=0,
                                               channel_multiplier=1),
             reads=["identf"], writes=["identf"])
        S.op("dve", lambda v: v.tensor_copy(out=ident[:], in_=identf[:]), reads=["identf"], writes=["ident"])
        S.op("dve", lambda v: v.memset(ones[:], 1.0), writes=["ones"])
        S.dma("sp", lambda q: q.dma_start(out=cact[:], in_=cT_in), writes=["cact"])
        S.op("act", lambda a: a.activation(out=cact[:], in_=cact[:], func=AF.Silu), reads=["cact"], writes=["cact"])
        S.op("dve", lambda v: v.tensor_copy(out=cbc[:], in_=cact[:].unsqueeze(2).to_broadcast([128, KC, 128])),
             reads=["cact"], writes=["cbc"])
        if do("attn1"):
            S.dma("sp", lambda q: q.dma_start(out=sinkbc[:], in_=sw_sinks.partition_broadcast(128)), writes=["sinkbc"])

        def bcast_row(dst, row_ap, width, pst, pname):
            S.dma("sp", lambda q: q.dma_start(out=rowt[0:1, 0:width], in_=row_ap), writes=["rowt"])
            for j in range(width // 512):
                S.op("pe", lambda pe, j=j: pe.matmul(pst[:, 0:512], lhsT=ones[0:1, :], rhs=rowt[0:1, j * 512:(j + 1) * 512],
                                                     start=True, stop=True),
                     reads=["rowt", "ones"], writes=[pname])
                S.op("act", lambda a, j=j: a.activation(out=dst[:, j * 512:(j + 1) * 512], in_=pst[:, 0:512], func=AF.Copy),
                     reads=[pname], writes=["bc_dst"])

        def mod_phase(l):
            with ExitStack() as _st:
                gbc = _st.enter_context(sb("gbc", [128, 2, D], F32))
                adaw = _st.enter_context(sb("adaw", [128, 2, KC, 512], F32))
                pm = _st.enter_context(ps("pm", [128, 2, 512], F32))
                for i in range(2):
                    bcast_row(gbc[:, i, :], norm_g[l, i:i + 1, :], D, pm[:, 0, :], "pm0")
                wv = ada_w[l].rearrange("(k p) n -> p k n", p=128)
                for cg in range(12):
                    b = cg % 2
                    S.dma("sp", lambda q, cg=cg, b=b: q.dma_start(out=adaw[:, b], in_=wv[:, :, cg * 512:(cg + 1) * 512]),
                          writes=["adaw%d" % b])
                    S.dma("sp", lambda q, cg=cg, b=b: q.dma_start(out=rowb[0:1, b, :], in_=ada_b[l:l + 1, cg * 512:(cg + 1) * 512]),
                          writes=["rowb%d" % b])

                    def f(pe, cg=cg, b=b):
                        for k in range(KC):
                            pe.matmul(pm[:, b, :], lhsT=cbc[:, k, :], rhs=adaw[:, b, k, :], start=(k == 0), stop=False)
                        return pe.matmul(pm[:, b, :], lhsT=ones[0:1, :], rhs=rowb[0:1, b, :],
                                         start=False, stop=True)
                    S.op("pe", f, reads=["adaw%d" % b, "cbc", "rowb%d" % b, "ones"], writes=["pm%d" % b])
                    j, hf = cg // 2, cg % 2
                    dst = modt[:, j, hf * 512:(hf + 1) * 512]
                    if j in (1, 4):
                        gsl = gbc[:, 0 if j == 1 else 1, hf * 512:(hf + 1) * 512]
                        S.op("dve", lambda v, dst=dst, b=b, gsl=gsl: v.scalar_tensor_tensor(
                            out=dst, in0=pm[:, b, :], scalar=1.0, in1=gsl, op0=ALU.add, op1=ALU.mult),
                            reads=["pm%d" % b, "bc_dst"], writes=["modt"])
                    else:
                        S.op("act", lambda a, dst=dst, b=b: a.activation(out=dst, in_=pm[:, b, :], func=AF.Copy),
                             reads=["pm%d" % b], writes=["modt"])
                S.barrier()

        def rms_stats(xv, xt, sqj, ssq, rstd, xs_dep):
            for t in range(NT):
                b = t % 2
                S.dma("sp", lambda q, t=t, b=b: q.dma_start(out=xt[:, b, :], in_=xv[t]), reads=xs_dep, writes=["xt%d" % b])
                S.op("act", lambda a, t=t, b=b: a.activation(out=sqj, in_=xt[:, b, :], func=AF.Square, accum_out=ssq[:, t:t + 1]),
                     reads=["xt%d" % b], writes=["ssq%d" % t, "sqj"])
            S.op("dve", lambda v: v.tensor_scalar(out=rstd, in0=ssq, scalar1=1.0 / D, scalar2=EPS, op0=ALU.mult, op1=ALU.add),
                 reads=["ssq%d" % t for t in range(NT)], writes=["rstd"])
            S.op("act", lambda a: a.activation(out=rstd, in_=rstd, func=AF.Sqrt), reads=["rstd"], writes=["rstd"])
            S.op("dve", lambda v: v.reciprocal(out=rstd, in_=rstd), reads=["rstd"], writes=["rstd"])

        def attn_phase(l):
            is_na = (l == 0)
            xin = x_in if l == 0 else xs
            wqkv_d = na_wqkv if is_na else sw_wqkv
            wo_d = na_wo if is_na else sw_wo
            bias_w = NA_TTOT if is_na else 384
            with ExitStack() as _st:
                hT = _st.enter_context(sb("hT", [128, KC, S_LEN], BF16))
                attnT = _st.enter_context(sb("attnT", [128, KC, S_LEN], BF16))
                qT = _st.enter_context(sb("qT", [128, S_LEN], BF16))
                kT = _st.enter_context(sb("kT", [128, S_LEN], BF16))
                vv = _st.enter_context(sb("vv", [128, NT, 128], BF16))
                wqkv = _st.enter_context(sb("wqkv", [128, 2, KC, 3, 128], BF16))
                wo = _st.enter_context(sb("wo", [128, KC, D], BF16))
                biast = _st.enter_context(sb("biast", [128, 2, bias_w], F32))
                oall = _st.enter_context(sb("oall", [128, NT, 128], BF16))
                xt = _st.enter_context(sb("xt", [128, 2, D], F32))
                tmpn = _st.enter_context(sb("tmpn", [128, 2, D], F32))
                sqj = _st.enter_context(sb("sqj", [128, D], BF16))
                ssq = _st.enter_context(sb("ssq", [128, NT], F32))
                rstd = _st.enter_context(sb("rstd", [128, NT], F32))
                hb = _st.enter_context(sb("hb", [128, 2, D], BF16))
                ssb = _st.enter_context(sb("ssb", [128, 2, 576], F32))
                pp = _st.enter_context(sb("pp", [128, 2, 576], BF16))
                pTs = _st.enter_context(sb("pTs", [128, 2, 5, 128], BF16))
                st2 = _st.enter_context(sb("st2", [128, 8, 8], F32))
                pS = _st.enter_context(ps("pS", [128, 2, 2, 512], F32))
                pT = _st.enter_context(ps("pT", [128, 2, 8, 128], BF16))
                pVO = _st.enter_context(ps("pVO", [128, 2, 512], F32))
                pV = pVO[:, 0, :].rearrange("p (a b) -> p a b", a=4)
                pO = pVO[:, :, 0:64]
                S.dma("pool", lambda g: g.dma_start(out=wo[:], in_=wo_d.rearrange("(k p) n -> p k n", p=128)), writes=["wo"])
                xv = xin.rearrange("(t p) d -> t p d", p=128)
                xs_dep = ["xs"] if l == 1 else []
                rms_stats(xv, xt, sqj[:], ssq[:], rstd[:], xs_dep)
                for t in range(NT):
                    b = t % 2
                    S.dma("sp", lambda q, t=t, b=b: q.dma_start(out=xt[:, b, :], in_=xv[t]), reads=xs_dep, writes=["xt%d" % b])
                    S.op("dve", lambda v, t=t, b=b: v.scalar_tensor_tensor(out=tmpn[:, b, :], in0=xt[:, b, :], scalar=rstd[:, t:t + 1],
                                                                           in1=modt[:, 1, :], op0=ALU.mult, op1=ALU.mult),
                         reads=["xt%d" % b, "rstd", "modt"], writes=["tmpn%d" % b])
                    S.op("dve", lambda g, b=b: g.tensor_tensor(out=hb[:, b, :], in0=tmpn[:, b, :], in1=modt[:, 0, :], op=ALU.add),
                         reads=["tmpn%d" % b, "modt"], writes=["hb%d" % b])

                    def f(pe, b=b):
                        for k in range(KC):
                            ins = pe.transpose(out=pT[:, b, k, :], in_=hb[:, b, k * 128:(k + 1) * 128], identity=ident[:])
                        return ins
                    S.op("pe", f, reads=["hb%d" % b, "ident"], writes=["pT%d" % b])
                    S.op("act", lambda a, t=t, b=b: a.activation(out=hT[:, :, t * 128:(t + 1) * 128], in_=pT[:, b, :, :], func=AF.Copy),
                         reads=["pT%d" % b], writes=["hT"])
                wsrc = wqkv_d.rearrange("(k p) n -> p k n", p=128)
                import os
                for hp in range(int(os.environ.get("DBG_NPAIRS", "8"))):
                    wb = hp % 2
                    wres = "wqkv%d" % wb
                    if is_na:
                        for j in range(3):
                            S.dma("pool", lambda g, j=j, hp=hp, wb=wb: g.dma_start(
                                out=wqkv[:, wb, :, j, :], in_=wsrc[:, :, j * D + hp * 128: j * D + (hp + 1) * 128]), writes=[wres])
                    else:
                        kvh = hp // 2
                        S.dma("pool", lambda g, hp=hp, wb=wb: g.dma_start(
                            out=wqkv[:, wb, :, 0, :], in_=wsrc[:, :, hp * 128:(hp + 1) * 128]), writes=[wres])
                        for j in (1, 2):
                            c0 = 1024 + (j - 1) * 256 + kvh * 64
                            for hh in range(2):
                                S.dma("pool", lambda g, j=j, hh=hh, wb=wb, c0=c0: g.dma_start(
                                    out=wqkv[:, wb, :, j, hh * 64:(hh + 1) * 64], in_=wsrc[:, :, c0:c0 + 64]), writes=[wres])
                    bsrc = na_bias if is_na else sw_bias

                    def load_bias(hp_, hh_):
                        S.dma("sp", lambda q: q.dma_start(out=biast[:, hh_, :], in_=bsrc[2 * hp_ + hh_]), writes=["bias%d" % hh_])
                    if hp == 0:
                        load_bias(0, 0)
                        load_bias(0, 1)
                    for j, dstT in ((0, qT), (1, kT)):
                        for tg in range(4):
                            pi = (j * 4 + tg) % 4
                            pst = pS[:, pi // 2, pi % 2, :]

                            def f(pe, j=j, tg=tg, pst=pst, wb=wb):
                                for k in range(KC):
                                    ins = pe.matmul(pst, lhsT=wqkv[:, wb, k, j, :], rhs=hT[:, k, tg * 512:(tg + 1) * 512],
                                                    start=(k == 0), stop=(k == KC - 1))
                                return ins
                            S.op("pe", f, reads=[wres, "hT"], writes=["pS%d" % pi])
                            S.op("act", lambda a, j=j, tg=tg, pst=pst, dstT=dstT: a.activation(
                                out=dstT[:, tg * 512:(tg + 1) * 512], in_=pst, func=AF.Copy, scale=(0.125 if j == 0 else 1.0)),
                                reads=["pS%d" % pi], writes=["qT" if j == 0 else "kT"])
                    for t4 in range(4):
                        def f(pe, t4=t4, wb=wb):
                            for tt in range(4):
                                t = t4 * 4 + tt
                                for k in range(KC):
                                    ins = pe.matmul(pV[:, tt, :], lhsT=hT[:, k, t * 128:(t + 1) * 128], rhs=wqkv[:, wb, k, 2, :],
                                                    start=(k == 0), stop=(k == KC - 1))
                            return ins
                        S.op("pe", f, reads=[wres, "hT"], writes=["pO0"])
                        S.op("dve", lambda v, t4=t4: v.tensor_copy(out=vv[:, t4 * 4:(t4 + 1) * 4, :], in_=pV),
                             reads=["pO0"], writes=["vv"])
                    iters = []
                    for hh in range(2):
                        for qb in range(NT):
                            if is_na:
                                r0 = 2 * qb
                                if r0 <= 2:
                                    krow, tt_ = 0, r0 // 2
                                elif r0 >= 28:
                                    krow, tt_ = 24, 3 + (r0 - 28) // 2
                                else:
                                    krow, tt_ = r0 - 4, 2
                                nk = NA_TW[tt_]
                                boff = NA_TOFF[tt_]
                                ks = krow * 64
                            else:
                                t_lo, t_hi = max(qb - 1, 0), min(qb + 1, NT - 1)
                                nk = (t_hi - t_lo + 1) * 128
                                ks = t_lo * 128
                                boff = 128 if qb == 0 else 0
                            iters.append(dict(hh=hh, qb=qb, nk=nk, boff=boff, ks=ks, half=nk // 2, kt0=ks // 128,
                                              nfull=nk // 128, rem=nk % 128, h=2 * hp + hh, hsl=slice(hh * 64, (hh + 1) * 64)))

                    def stA(j, it):
                        sb_ = j % 2

                        def f(pe):
                            for pc in range(2):
                                ins = pe.matmul(pS[:, sb_, pc, 0:it["half"]], lhsT=qT[it["hsl"], it["qb"] * 128:(it["qb"] + 1) * 128],
                                                rhs=kT[it["hsl"], it["ks"] + pc * it["half"]: it["ks"] + (pc + 1) * it["half"]],
                                                start=True, stop=True)
                            return ins
                        S.op("pe", f, reads=["qT", "kT"], writes=["pS%d" % (2 * sb_), "pS%d" % (2 * sb_ + 1)])

                    def stB(j, it):
                        sb_ = j % 2
                        nk, half, boff, hh, h = it["nk"], it["half"], it["boff"], it["hh"], it["h"]
                        stt = st2[:, j % 8, :]
                        sres = "st2_%d" % (j % 8)
                        S.op("dve", lambda v: v.tensor_tensor(
                            out=ssb[:, sb_, 0:nk].rearrange("p (c n) -> p c n", c=2), in0=pS[:, sb_, :, 0:half],
                            in1=biast[:, hh, boff:boff + nk].rearrange("p (c n) -> p c n", c=2), op=ALU.add),
                            reads=["pS%d" % (2 * sb_), "pS%d" % (2 * sb_ + 1), "bias%d" % hh], writes=["ssb%d" % sb_])
                        if is_na:
                            S.op("dve", lambda v: v.tensor_reduce(out=stt[:, 0:1], in_=ssb[:, sb_, 0:nk], axis=AX.X, op=ALU.max, negate=True),
                                 reads=["ssb%d" % sb_], writes=[sres])
                        else:
                            S.op("dve", lambda v: v.tensor_reduce(out=stt[:, 4:5], in_=ssb[:, sb_, 0:nk], axis=AX.X, op=ALU.max),
                                 reads=["ssb%d" % sb_], writes=[sres])
                            S.op("dve", lambda v: v.tensor_scalar(out=stt[:, 0:1], in0=stt[:, 4:5], scalar1=sinkbc[:, h:h + 1],
                                                                  scalar2=-1.0, op0=ALU.max, op1=ALU.mult),
                                 reads=[sres, "sinkbc"], writes=[sres])

                    def stC(j, it):
                        sb_ = j % 2
                        nk, h = it["nk"], it["h"]
                        stt = st2[:, j % 8, :]
                        sres = "st2_%d" % (j % 8)
                        S.op("act", lambda a: a.activation(out=pp[:, sb_, 0:nk], in_=ssb[:, sb_, 0:nk], func=AF.Exp, bias=stt[:, 0:1],
                                                           scale=1.0, accum_out=stt[:, 1:2]),
                             reads=["ssb%d" % sb_, sres], writes=["pp%d" % sb_, sres + "s"])
                        if not is_na:
                            S.op("act", lambda a: a.activation(out=stt[:, 5:6], in_=sinkbc[:, h:h + 1], func=AF.Exp, bias=stt[:, 0:1], scale=1.0),
                                 reads=[sres, "sinkbc"], writes=[sres + "e"])

                    def stD(j, it):
                        sb_ = j % 2
                        nfull, rem = it["nfull"], it["rem"]
                        stt = st2[:, j % 8, :]
                        sres = "st2_%d" % (j % 8)

                        def f(pe):
                            for c in range(nfull):
                                ins = pe.transpose(out=pT[:, sb_, c, :], in_=pp[:, sb_, c * 128:(c + 1) * 128], identity=ident[:])
                            if rem:
                                ins = pe.transpose(out=pT[0:rem, sb_, nfull, :], in_=pp[:, sb_, nfull * 128: nfull * 128 + rem], identity=ident[:])
                            return ins
                        S.op("pe", f, reads=["pp%d" % sb_, "ident"], writes=["pT%d" % sb_])
                        if not is_na:
                            S.op("dve", lambda v: v.tensor_tensor(out=stt[:, 1:2], in0=stt[:, 1:2], in1=stt[:, 5:6], op=ALU.add),
                                 reads=[sres + "s", sres + "e"], writes=[sres + "s"])
                        S.op("dve", lambda v: v.reciprocal(out=stt[:, 2:3], in_=stt[:, 1:2]), reads=[sres + "s"], writes=[sres + "r"])

                    def stE(j, it):
                        sb_ = j % 2
                        nfull, rem = it["nfull"], it["rem"]
                        S.op("act", lambda a: a.activation(out=pTs[:, sb_, 0:nfull, :], in_=pT[:, sb_, 0:nfull, :], func=AF.Copy),
                             reads=["pT%d" % sb_], writes=["pTs%d" % sb_])
                        if rem:
                            S.op("act", lambda a: a.activation(out=pTs[0:rem, sb_, nfull, :], in_=pT[0:rem, sb_, nfull, :], func=AF.Copy),
                                 reads=["pT%d" % sb_], writes=["pTs%d" % sb_])

                    def stF(j, it):
                        sb_ = j % 2
                        nfull, rem, kt0, hsl = it["nfull"], it["rem"], it["kt0"], it["hsl"]

                        def f(pe):
                            tot = nfull + (1 if rem else 0)
                            for c in range(nfull):
                                ins = pe.matmul(pO[:, sb_, :], lhsT=pTs[:, sb_, c, :], rhs=vv[:, kt0 + c, hsl], start=(c == 0), stop=(c == tot - 1))
                            if rem:
                                ins = pe.matmul(pO[:, sb_, :], lhsT=pTs[0:rem, sb_, nfull, :], rhs=vv[0:rem, kt0 + nfull, hsl], start=False, stop=True)
                            return ins
                        S.op("pe", f, reads=["pTs%d" % sb_, "vv"], writes=["pO%d" % sb_])

                    def stG(j, it):
                        sb_ = j % 2
                        stt = st2[:, j % 8, :]
                        sres = "st2_%d" % (j % 8)
                        S.op("act", lambda a: a.activation(out=oall[:, it["qb"], it["hsl"]], in_=pO[:, sb_, :], func=AF.Copy, scale=stt[:, 2:3]),
                             reads=["pO%d" % sb_, sres + "r"], writes=["oall"])

                    stages = [stA, stB, stC, stD, stE, stF, stG][:int(os.environ.get("DBG_NST", "7"))]
                    n_it = min(len(iters), int(os.environ.get("DBG_NIT", "99")))
                    SKEW = True
                    for step in range(n_it + (len(stages) - 1 if SKEW else 0)):
                        for si in (range(len(stages) - 1, -1, -1) if SKEW else range(len(stages))):
                            j = step - si if SKEW else step
                            if 0 <= j < n_it:
                                stages[si](j, iters[j])
                        if hp + 1 < 8 and step == NT:
                            load_bias(hp + 1, 0)
                    if hp + 1 < 8:
                        load_bias(hp + 1, 1)
                    for t4 in range(4):
                        b = t4 % 2

                        def f(pe, t4=t4, b=b):
                            for tt in range(4):
                                ins = pe.transpose(out=pT[:, b, tt, :], in_=oall[:, t4 * 4 + tt, :], identity=ident[:])
                            return ins
                        S.op("pe", f, reads=["oall", "ident"], writes=["pT%d" % b])
                        S.op("dve", lambda v, t4=t4, b=b, hp=hp: v.tensor_copy(
                            out=attnT[:, hp, t4 * 512:(t4 + 1) * 512].rearrange("p (t n) -> p t n", t=4), in_=pT[:, b, 0:4, :]),
                            reads=["pT%d" % b], writes=["attnT"])
                xo = xs.rearrange("(t p) d -> t p d", p=128)
                for t in range(NT):
                    b = t % 2
                    S.dma("sp", lambda q, t=t, b=b: q.dma_start(out=xt[:, b, :], in_=xv[t]), reads=xs_dep, writes=["xt%d" % b])

                    def f(pe, t=t, b=b):
                        for n in range(2):
                            for k in range(KC):
                                ins = pe.matmul(pS[:, b, n, :], lhsT=attnT[:, k, t * 128:(t + 1) * 128], rhs=wo[:, k, n * 512:(n + 1) * 512],
                                                start=(k == 0), stop=(k == KC - 1))
                        return ins
                    S.op("pe", f, reads=["attnT", "wo"], writes=["pS%d" % (2 * b), "pS%d" % (2 * b + 1)])
                    S.op("dve", lambda v, b=b: v.tensor_tensor(out=tmpn[:, b, :].rearrange("p (n c) -> p n c", n=2), in0=pS[:, b, :, :],
                                                                in1=modt[:, 2, :].rearrange("p (n c) -> p n c", n=2), op=ALU.mult),
                         reads=["pS%d" % (2 * b), "pS%d" % (2 * b + 1), "modt"], writes=["tmpn%d" % b])
                    S.op("dve", lambda g, b=b: g.tensor_tensor(out=xt[:, b, :], in0=xt[:, b, :], in1=tmpn[:, b, :], op=ALU.add),
                         reads=["tmpn%d" % b, "xt%d" % b], writes=["xt%d" % b])
                    S.dma("sp", lambda q, t=t, b=b: q.dma_start(out=xo[t], in_=xt[:, b, :]), reads=["xt%d" % b], writes=["xs"])
                S.barrier()

        def moe_phase(l):
            RING = 12
            with sb("ring", [128, RING, 4096], BF16) as ring:
                chunk_no = [0]

                def load_chunk(kind, e, i):
                    slot = chunk_no[0] % RING
                    chunk_no[0] += 1
                    res = "ring%d" % slot
                    if kind == "down":
                        src = w_down[l, e, i * 512:(i + 1) * 512, :].rearrange("(j p) n -> p j n", p=128)
                        dst = ring[:, slot, :].rearrange("p (j n) -> p j n", j=4)
                    else:
                        wsrc = w_gate if kind == "gate" else w_up
                        src = wsrc[l, e, :, i * 512:(i + 1) * 512].rearrange("(k p) n -> p k n", p=128)
                        dst = ring[:, slot, :].rearrange("p (k n) -> p k n", k=KC)
                    S.dma("pool", lambda g: g.dma_start(out=dst, in_=src), writes=[res])
                    return slot

                def load_expert(e):
                    sl = {}
                    for fg in range(4):
                        sl[("gate", fg)] = load_chunk("gate", e, fg)
                        sl[("up", fg)] = load_chunk("up", e, fg)
                    for i in range(4):
                        sl[("down", i)] = load_chunk("down", e, i)
                    return sl

                slots0 = load_expert(0)
                with ExitStack() as _st:
                    idxT = _st.enter_context(sb("idxT", [128, 2, NE], U32))
                    valsT = _st.enter_context(sb("valsT", [128, 2, NE], F32))
                    with ExitStack() as _st:
                        xt = _st.enter_context(sb("xt", [128, 2, D], F32))
                        tmpn = _st.enter_context(sb("tmpn", [128, 2, D], F32))
                        sqj = _st.enter_context(sb("sqj", [128, D], BF16))
                        ssq = _st.enter_context(sb("ssq", [128, NT], F32))
                        rstd = _st.enter_context(sb("rstd", [128, NT], F32))
                        rst = _st.enter_context(sb("rst", [128, 3, NT], F32))
                        lg2 = _st.enter_context(sb("lg2", [128, 2, NE], F32))
                        h2f = _st.enter_context(sb("h2f", [128, 2, D], F32))
                        h2b = _st.enter_context(sb("h2b", [128, 2, D], BF16))
                        h2T = _st.enter_context(sb("h2T", [128, 2, KC, 128], F32))
                        wr = _st.enter_context(sb("wr", [128, KC, NE], F32))
                        stat = _st.enter_context(sb("stat", [128, 8], F32))
                        lg = _st.enter_context(sb("lg", [128, 2, NE], F32))
                        cur = _st.enter_context(sb("cur", [16, 2, S_LEN], F32))
                        vals = _st.enter_context(sb("vals", [16, CAP], F32))
                        idx = _st.enter_context(sb("idx", [16, CAP], U32))
                        idxf = _st.enter_context(sb("idxf", [16, CAP], F32))
                        pF = _st.enter_context(ps("pF", [128, 2, 2, 4, 128], F32))
                        pL = _st.enter_context(ps("pL", [128, 2, 512], F32))
                        pA = _st.enter_context(ps("pA", [128, 2, 512], F32))
                        pI = pA[:, 0, 0:2 * NE].rearrange("p (a b) -> p a b", a=2)
                        S.dma("sp", lambda q: q.dma_start(out=wr[:], in_=w_router[l].rearrange("(k p) n -> p k n", p=128)), writes=["wr"])
                        xv = xs.rearrange("(t p) d -> t p d", p=128)
                        hv = hs.rearrange("(t p) d -> t p d", p=128)
                        rms_stats(xv, xt, sqj[:], ssq[:], rstd[:], ["xs"])

                        def r1(t):
                            b = t % 2
                            S.dma("sp", lambda q: q.dma_start(out=xt[:, b, :], in_=xv[t]), reads=["xs"], writes=["xt%d" % b])
                            S.op("dve", lambda v: v.scalar_tensor_tensor(out=tmpn[:, b, :], in0=xt[:, b, :], scalar=rstd[:, t:t + 1],
                                                                         in1=modt[:, 4, :], op0=ALU.mult, op1=ALU.mult),
                                 reads=["xt%d" % b, "rstd", "modt"], writes=["tmpn%d" % b])
                            S.op("dve", lambda v: v.tensor_tensor(out=h2f[:, b, :], in0=tmpn[:, b, :], in1=modt[:, 3, :], op=ALU.add),
                                 reads=["tmpn%d" % b, "modt"], writes=["h2f%d" % b])
                            S.op("act", lambda a: a.activation(out=h2b[:, b, :], in_=h2f[:, b, :], func=AF.Copy),
                                 reads=["h2f%d" % b], writes=["h2b%d" % b])
                            S.dma("sp", lambda q: q.dma_start(out=hv[t], in_=h2b[:, b, :]), reads=["h2b%d" % b], writes=["hs"])
                            for g2 in range(2):
                                def f(pe, g2=g2):
                                    for kk in range(4):
                                        k = g2 * 4 + kk
                                        ins = pe.transpose(out=pF[:, b, g2, kk, :], in_=h2f[:, b, k * 128:(k + 1) * 128], identity=identf[:])
                                    return ins
                                S.op("pe", f, reads=["h2f%d" % b, "identf"], writes=["pF%d_%d" % (b, g2)])

                        def r2(t):
                            b = t % 2
                            for g2 in range(2):
                                S.op("act", lambda a, g2=g2: a.activation(out=h2T[:, b, g2 * 4:(g2 + 1) * 4, :], in_=pF[:, b, g2, :, :], func=AF.Copy),
                                     reads=["pF%d_%d" % (b, g2)], writes=["h2T%d" % b])

                            def f(pe):
                                for k in range(KC):
                                    ins = pe.matmul(pL[:, b, 0:NE], lhsT=h2T[:, b, k, :], rhs=wr[:, k, :], start=(k == 0), stop=(k == KC - 1))
                                return ins
                            S.op("pe", f, reads=["h2T%d" % b, "wr"], writes=["pL%d" % b])
                            S.op("dve", lambda v: v.tensor_reduce(out=rst[:, 0, t:t + 1], in_=pL[:, b, 0:NE], axis=AX.X, op=ALU.max, negate=True),
                                 reads=["pL%d" % b], writes=["rm%d" % t])

                        def r3(t):
                            b = t % 2
                            S.op("act", lambda a: a.activation(out=lg[:, b, :], in_=pL[:, b, 0:NE], func=AF.Exp, bias=rst[:, 0, t:t + 1], scale=1.0,
                                                               accum_out=rst[:, 1, t:t + 1]),
                                 reads=["pL%d" % b, "rm%d" % t], writes=["lg%d" % b, "rs%d" % t])
                            S.op("dve", lambda v: v.reciprocal(out=rst[:, 2, t:t + 1], in_=rst[:, 1, t:t + 1]), reads=["rs%d" % t], writes=["rr%d" % t])
                            S.op("dve", lambda v: v.tensor_scalar(out=lg2[:, b, :], in0=lg[:, b, :], scalar1=rst[:, 2, t:t + 1], scalar2=None,
                                                                  op0=ALU.mult), reads=["lg%d" % b, "rr%d" % t], writes=["lg2_%d" % b])
                            S.op("pe", lambda pe: pe.transpose(out=pA[0:NE, b, 0:128], in_=lg2[:, b, :], identity=identf[:]),
                                 reads=["lg2_%d" % b, "identf"], writes=["pA%d" % b])

                        def r4(t):
                            b = t % 2
                            S.op("act", lambda a: a.activation(out=cur[:, 0, t * 128:(t + 1) * 128], in_=pA[0:NE, b, 0:128], func=AF.Copy),
                                 reads=["pA%d" % b], writes=["cur0"])

                        rstages = [r1, r2, r3, r4]
                        for step in range(NT + len(rstages) - 1):
                            for si in range(len(rstages) - 1, -1, -1):
                                t = step - si
                                if 0 <= t < NT:
                                    rstages[si](t)
                        for r in range(CAP // 8):
                            c, n = r % 2, (r + 1) % 2
                            rs_ = slice(r * 8, (r + 1) * 8)
                            S.op("dve", lambda v, rs_=rs_, c=c: v.max(out=vals[:, rs_], in_=cur[:, c, :]), reads=["cur%d" % c], writes=["vals"])
                            S.op("dve", lambda v, rs_=rs_, c=c: v.max_index(out=idx[:, rs_], in_max=vals[:, rs_], in_values=cur[:, c, :]),
                                 reads=["cur%d" % c, "vals"], writes=["idx"])
                            S.op("dve", lambda v, rs_=rs_, c=c, n=n: v.match_replace(out=cur[:, n, :], in_to_replace=vals[:, rs_],
                                                                                    in_values=cur[:, c, :], imm_value=-1.0),
                                 reads=["cur%d" % c, "vals"], writes=["cur%d" % n])
                        S.op("dve", lambda v: v.tensor_copy(out=idxf[:], in_=idx[:]), reads=["idx"], writes=["idxf"])
                        for src_t, dst_t, nm in ((idxf, idxT, "idxT"), (vals, valsT, "valsT")):
                            def f(pe, src_t=src_t):
                                for hh in range(2):
                                    ins = pe.transpose(out=pI[:, hh, :], in_=src_t[:, hh * 128:(hh + 1) * 128], identity=identf[0:16, 0:16])
                                return ins
                            S.op("pe", f, reads=["idxf", "vals", "identf"], writes=["pA0"])
                            S.op("dve", lambda v, dst_t=dst_t: v.tensor_copy(out=dst_t[:], in_=pI), reads=["pA0"], writes=[nm])
                        S.barrier()
                    with ExitStack() as _st:
                        xg = _st.enter_context(sb("xg", [128, 2, 2, D], BF16))
                        xgT = _st.enter_context(sb("xgT", [128, 2, KC, CAP], BF16))
                        actT = _st.enter_context(sb("actT", [128, 2, 16, CAP], BF16))
                        sg = _st.enter_context(sb("sg", [128, 2, CAP], F32))
                        ys = _st.enter_context(sb("ys", [128, 2, D], F32))
                        pX = _st.enter_context(ps("pX", [128, 2, KC, 128], BF16))
                        pG = _st.enter_context(ps("pG", [128, 2, 2, CAP], F32))
                        pY = _st.enter_context(ps("pY", [128, 2, 2, 512], F32))
                        slots = slots0

                        def gather(e):
                            eb = e % 2
                            for hf in range(2):
                                S.dma("pool", lambda g, e=e, hf=hf, eb=eb: g.indirect_dma_start(
                                    out=xg[:, eb, hf, :], out_offset=None, in_=hs,
                                    in_offset=bass.IndirectOffsetOnAxis(ap=idxT[:, hf, e:e + 1], axis=0)),
                                    reads=["idxT", "hs"], writes=["xg%d_%d" % (eb, hf)])
                        gather(0)
                        for e in range(NE):
                            eb = e % 2
                            for hf in range(2):
                                def f(pe, eb=eb, hf=hf):
                                    for k in range(KC):
                                        ins = pe.transpose(out=pX[:, hf, k, :], in_=xg[:, eb, hf, k * 128:(k + 1) * 128], identity=ident[:])
                                    return ins
                                S.op("pe", f, reads=["xg%d_%d" % (eb, hf), "ident"], writes=["pX%d" % hf])
                                S.op("act", lambda a, eb=eb, hf=hf: a.activation(out=xgT[:, eb, :, hf * 128:(hf + 1) * 128], in_=pX[:, hf, :, :], func=AF.Copy),
                                     reads=["pX%d" % hf], writes=["xgT%d" % eb])
                            fi_n = 0
                            for fg in range(4):
                                sg_ = slots[("gate", fg)]
                                su_ = slots[("up", fg)]
                                gch = ring[:, sg_, :].rearrange("p (k n) -> p k n", k=KC)
                                uch = ring[:, su_, :].rearrange("p (k n) -> p k n", k=KC)
                                for fs in range(4):
                                    fi = fg * 4 + fs
                                    pb = fi % 2

                                    def f(pe, gch=gch, uch=uch, fs=fs, pb=pb, eb=eb):
                                        for j, ch in ((0, gch), (1, uch)):
                                            for k in range(KC):
                                                ins = pe.matmul(pG[:, pb, j, :], lhsT=ch[:, k, fs * 128:(fs + 1) * 128], rhs=xgT[:, eb, k, :],
                                                                start=(k == 0), stop=(k == KC - 1))
                                        return ins
                                    S.op("pe", f, reads=["ring%d" % sg_, "ring%d" % su_, "xgT%d" % eb], writes=["pG%d" % pb])
                                    S.op("act", lambda a, pb=pb: a.activation(out=sg[:, pb, :], in_=pG[:, pb, 0, :], func=AF.Silu),
                                         reads=["pG%d" % pb], writes=["sg%d" % pb])
                                    S.op("dve", lambda v, pb=pb, fi=fi, eb=eb: v.tensor_tensor(out=actT[:, eb, fi, :], in0=sg[:, pb, :], in1=pG[:, pb, 1, :],
                                                                                              op=ALU.mult),
                                         reads=["sg%d" % pb, "pG%d" % pb], writes=["actT%d" % eb])
                            for tt in range(2):
                                def f(pe, tt=tt, eb=eb, slots=slots):
                                    for n in range(2):
                                        for fi in range(16):
                                            dch = ring[:, slots[("down", fi // 4)], :].rearrange("p (j n) -> p j n", j=4)
                                            ins = pe.matmul(pY[:, tt, n, :], lhsT=actT[:, eb, fi, tt * 128:(tt + 1) * 128],
                                                            rhs=dch[:, fi % 4, n * 512:(n + 1) * 512], start=(fi == 0), stop=(fi == 15))
                                    return ins
                                S.op("pe", f, reads=["actT%d" % eb] + ["ring%d" % slots[("down", i)] for i in range(4)],
                                     writes=["pY%d" % tt])
                                S.op("dve", lambda v, tt=tt, e=e: v.scalar_tensor_tensor(
                                    out=ys[:, tt, :].rearrange("p (n c) -> p n c", n=2), in0=pY[:, tt, :, :], scalar=valsT[:, tt, e:e + 1],
                                    in1=modt[:, 5, :].rearrange("p (n c) -> p n c", n=2), op0=ALU.mult, op1=ALU.mult),
                                    reads=["pY%d" % tt, "valsT", "modt"], writes=["ys%d" % tt])
                            if e + 1 < NE:
                                slots_next = load_expert(e + 1)
                                gather(e + 1)
                            for tt in range(2):
                                S.dma("pool", lambda g, tt=tt, e=e: g.indirect_dma_start(
                                    out=xs, out_offset=bass.IndirectOffsetOnAxis(ap=idxT[:, tt, e:e + 1], axis=0),
                                    in_=ys[:, tt, :], in_offset=None, compute_op=ALU.add),
                                    reads=["ys%d" % tt, "idxT", "xs"], writes=["xs"])
                            if e + 1 < NE:
                                slots = slots_next
                        S.barrier()

        def final_phase():
            with ExitStack() as _st:
                fgb = _st.enter_context(sb("fgb", [128, D], F32))
                xt = _st.enter_context(sb("xt", [128, 2, D], F32))
                sqj = _st.enter_context(sb("sqj", [128, D], BF16))
                ssq = _st.enter_context(sb("ssq", [128, NT], F32))
                rstd = _st.enter_context(sb("rstd", [128, NT], F32))
                ot = _st.enter_context(sb("ot", [128, 2, D], F32))
                pm = _st.enter_context(ps("pm", [128, 512], F32))
                bcast_row(fgb[:], final_g, D, pm[:], "pmf")
                xv = xs.rearrange("(t p) d -> t p d", p=128)
                ov = out.rearrange("(t p) d -> t p d", p=128)
                rms_stats(xv, xt, sqj[:], ssq[:], rstd[:], ["xs"])
                for t in range(NT):
                    b = t % 2
                    S.dma("sp", lambda q, t=t, b=b: q.dma_start(out=xt[:, b, :], in_=xv[t]), reads=["xs"], writes=["xt%d" % b])
                    S.op("dve", lambda v, t=t, b=b: v.scalar_tensor_tensor(out=ot[:, b, :], in0=xt[:, b, :], scalar=rstd[:, t:t + 1], in1=fgb[:],
                                                                           op0=ALU.mult, op1=ALU.mult),
                         reads=["xt%d" % b, "rstd", "bc_dst"], writes=["ot%d" % b])
                    S.dma("sp", lambda q, t=t, b=b: q.dma_start(out=ov[t], in_=ot[:, b, :]), reads=["ot%d" % b], writes=["out"])

        def copy_out():
            with sb("xt", [128, 2, D], F32) as xt:
                xv = xs.rearrange("(t p) d -> t p d", p=128)
                ov = out.rearrange("(t p) d -> t p d", p=128)
                for t in range(NT):
                    b = t % 2
                    S.dma("sp", lambda q, t=t, b=b: q.dma_start(out=xt[:, b, :], in_=xv[t]), reads=["xs"], writes=["xt%d" % b])
                    S.dma("sp", lambda q, t=t, b=b: q.dma_start(out=ov[t], in_=xt[:, b, :]), reads=["xt%d" % b], writes=["out"])

        for l in range(2):
            if not do("attn%d" % l):
                break
            mod_phase(l)
            attn_phase(l)
            if do("moe%d" % l) and not skip_moe:
                moe_phase(l)
        if do("final"):
            final_phase()
        else:
            copy_out()
        for e in Sched.ENG:
            S.wait_all_on(e)
    S.close()
    return nc, S


def _t5_buckets(rel):
    half, max_exact = 16, 8
    n = np.abs(rel)
    large = max_exact + (np.log(np.maximum(n, 1) / max_exact) / np.log(128 / max_exact) * (half - max_exact)).astype(np.int32)
    large = np.minimum(large, half - 1)
    return (rel > 0).astype(np.int32) * half + np.where(n < max_exact, n, large)


def _na_bias_tables(rpb):
    out = np.full((16, 128, NA_TTOT), NEG, np.float32)
    p = np.arange(128)
    rq, cq = p // 64, p % 64
    for tt_, r0 in enumerate([0, 2, 10, 28, 30]):
        if r0 <= 2:
            krow0 = 0
        elif r0 >= 28:
            krow0 = 24
        else:
            krow0 = r0 - 4
        nk = NA_TW[tt_]
        j = np.arange(nk)
        krow = krow0 + j // 64
        kcol = j % 64
        r = r0 + rq
        rs = np.clip(r - 4, 0, 24)
        cstart = np.clip(cq - 8, 0, 48)
        ok = ((krow[None, :] >= rs[:, None]) & (krow[None, :] < rs[:, None] + 8)
              & (kcol[None, :] >= cstart[:, None]) & (kcol[None, :] < cstart[:, None] + 16))
        dr = np.clip(krow[None, :] - r[:, None] + 7, 0, 14)
        dc = np.clip(kcol[None, :] - cq[:, None] + 15, 0, 30)
        vals = rpb[:, dr, dc]
        blk = np.where(ok[None], vals, np.float32(NEG)).astype(np.float32)
        out[:, :, NA_TOFF[tt_]:NA_TOFF[tt_] + nk] = blk
    return out


def _sw_bias_table(t5):
    rel = (np.arange(384)[None, :] - 128) - np.arange(128)[:, None]
    b = np.transpose(t5[_t5_buckets(rel)], (2, 0, 1))
    return np.where((np.abs(rel) <= 128)[None], b, np.float32(NEG)).astype(np.float32)


def make_in_maps(inputs, cores, stop_after="final", skip_moe=False):
    order = ["attn0", "moe0", "attn1", "moe1", "final"]
    do = lambda st: order.index(st) <= order.index(stop_after)
    f32 = lambda a: np.ascontiguousarray(np.asarray(a, dtype=np.float32))
    shared = {
        "ada_w": f32(inputs["ada_w"]), "ada_b": f32(inputs["ada_b"]), "norm_g": f32(inputs["norm_g"]),
        "na_w_qkv": f32(inputs["na_w_qkv"][0]), "na_w_o": f32(inputs["na_w_o"][0]),
        "na_bias": _na_bias_tables(f32(inputs["na_rpb"][0])),
    }
    if do("attn1"):
        shared.update({"sw_w_qkv": f32(inputs["sw_w_qkv"][0]), "sw_w_o": f32(inputs["sw_w_o"][0]),
                       "sw_bias": _sw_bias_table(f32(inputs["t5_bias"])), "sw_sinks": f32(inputs["sw_sinks"]).reshape(1, 16)})
    n_moe = 0 if skip_moe else (2 if do("moe1") else (1 if do("moe0") else 0))
    if n_moe:
        shared.update({"moe_w_router": f32(inputs["moe_w_router"][:n_moe]), "moe_w_gate": f32(inputs["moe_w_gate"][:n_moe]),
                       "moe_w_up": f32(inputs["moe_w_up"][:n_moe]), "moe_w_down": f32(inputs["moe_w_down"][:n_moe])})
    if do("final"):
        shared["final_g"] = f32(inputs["final_g"]).reshape(1, D)
    x = f32(inputs["x"])
    c = f32(inputs["c"])
    maps = []
    for b in cores:
        m = dict(shared)
        m["x"] = np.ascontiguousarray(x[b])
        m["cT"] = np.ascontiguousarray(c[b].reshape(KC, 128).T)
        maps.append(m)
    return maps


_PROG = {}


def kernel(**inputs):
    if "final" not in _PROG:
        _PROG["final"] = build_program("final")[0]
    nc = _PROG["final"]
    cores = list(range(8))
    in_maps = make_in_maps(inputs, cores)
    res = run_bass_kernel_spmd(nc, in_maps, core_ids=cores)
    return np.stack([np.asarray(r["out"], dtype=np.float32) for r in res.results], axis=0)
```

```python
import numpy as np
from contextlib import ExitStack
import concourse.bass as bass
import concourse.mybir as mybir
from concourse.bass_utils import run_bass_kernel_spmd

F32 = mybir.dt.float32
BF16 = mybir.dt.bfloat16
U32 = mybir.dt.uint32
AF = mybir.ActivationFunctionType
ALU = mybir.AluOpType
AX = mybir.AxisListType

S_LEN = 2048
D = 1024
NT = 16
KC = 8
NE = 16
FF = 2048
CAP = 256
EPS = 1e-6
NEG = -1e30
NA_TW = [512, 512, 576, 512, 512]
NA_TOFF = [0, 512, 1024, 1600, 2112]
NA_TTOT = 2624


class Sched:
    ENG = ("pe", "act", "dve", "pool", "sp")

    def __init__(self, nc, n_dma_sems=8):
        self.nc = nc
        self.eng = {"pe": nc.tensor, "act": nc.scalar, "dve": nc.vector,
                    "pool": nc.gpsimd, "sp": nc.sync}
        self._ctx = []
        self.csem = {}
        self.ccount = {e: 0 for e in self.ENG}
        for e in self.ENG:
            self.csem[e] = self._sem("c_" + e)
        self.dsem = {}
        self.dcount = {}
        self.dnext = {}
        for q in ("sp", "act", "pool"):
            self.dsem[q] = [self._sem("d_%s%d" % (q, i)) for i in range(n_dma_sems)]
            self.dcount[q] = [0] * n_dma_sems
            self.dnext[q] = 0
        self.last_write = {}
        self.readers = {}
        self.seen = {e: {} for e in self.ENG}
        self.n_wait = 0
        self.n_ops = 0

    def _sem(self, name):
        cm = self.nc.semaphore(name)
        s = cm.__enter__()
        self._ctx.append(cm)
        return s

    def _need(self, e, tok):
        kind, key, val, sem, src = tok
        if kind == "c" and src == e and e == "pe":
            return
        k = (kind, key)
        if self.seen[e].get(k, 0) >= val:
            return
        self.seen[e][k] = val
        self.eng[e].wait_ge(sem, val)
        self.n_wait += 1

    def _deps(self, e, reads, writes):
        toks = []
        for r in reads:
            t = self.last_write.get(r)
            if t is not None:
                toks.append(t)
        for w in writes:
            t = self.last_write.get(w)
            if t is not None:
                toks.append(t)
            toks.extend(self.readers.get(w, ()))
        for t in toks:
            self._need(e, t)

    def _commit(self, tok, reads, writes):
        for r in reads:
            self.readers.setdefault(r, []).append(tok)
        for w in writes:
            self.last_write[w] = tok
            self.readers[w] = []

    def op(self, e, fn, reads=(), writes=()):
        self._deps(e, reads, writes)
        ins = fn(self.eng[e])
        self.ccount[e] += 1
        ins.then_inc(self.csem[e], 1)
        tok = ("c", e, self.ccount[e], self.csem[e], e)
        self._commit(tok, reads, writes)
        self.n_ops += 1
        return tok

    def dma(self, q, fn, reads=(), writes=()):
        i = self.dnext[q]
        self.dnext[q] = (i + 1) % len(self.dsem[q])
        sem = self.dsem[q][i]
        if self.dcount[q][i] > 0:
            self._need(q, ("d", (q, i), self.dcount[q][i] * 16, sem, q))
        self._deps(q, reads, writes)
        ins = fn(self.eng[q])
        self.dcount[q][i] += 1
        ins.then_inc(sem, 16)
        tok = ("d", (q, i), self.dcount[q][i] * 16, sem, q)
        self._commit(tok, reads, writes)
        self.n_ops += 1
        return tok

    def wait_all_on(self, e):
        for f in self.ENG:
            if self.ccount[f] > 0:
                self._need(e, ("c", f, self.ccount[f], self.csem[f], f))
        for q in self.dsem:
            for i, sem in enumerate(self.dsem[q]):
                if self.dcount[q][i] > 0:
                    self._need(e, ("d", (q, i), self.dcount[q][i] * 16, sem, q))

    def barrier(self):
        for e in self.ENG:
            self.wait_all_on(e)
        self.last_write = {}
        self.readers = {}

    def close(self):
        for cm in reversed(self._ctx):
            cm.__exit__(None, None, None)


def build_program(stop_after="final", skip_moe=False):
    order = ["attn0", "moe0", "attn1", "moe1", "final"]
    last = order.index(stop_after)
    do = lambda st: order.index(st) <= last
    nc = bass.Bass("TRN2", target_bir_lowering=False)

    def din(name, shape, dt=F32):
        return nc.dram_tensor(name, list(shape), dt, kind="ExternalInput").ap()

    x_in = din("x", [S_LEN, D])
    cT_in = din("cT", [128, KC])
    ada_w = din("ada_w", [2, D, 6 * D])
    ada_b = din("ada_b", [2, 6 * D])
    norm_g = din("norm_g", [2, 2, D])
    na_wqkv = din("na_w_qkv", [D, 3 * D])
    na_wo = din("na_w_o", [D, D])
    na_bias = din("na_bias", [16, 128, NA_TTOT])
    if do("attn1"):
        sw_wqkv = din("sw_w_qkv", [D, 1536])
        sw_wo = din("sw_w_o", [D, D])
        sw_bias = din("sw_bias", [16, 128, 384])
        sw_sinks = din("sw_sinks", [1, 16])
    n_moe = 0 if skip_moe else (2 if do("moe1") else (1 if do("moe0") else 0))
    if n_moe:
        w_router = din("moe_w_router", [n_moe, D, NE])
        w_gate = din("moe_w_gate", [n_moe, NE, D, FF])
        w_up = din("moe_w_up", [n_moe, NE, D, FF])
        w_down = din("moe_w_down", [n_moe, NE, FF, D])
    if do("final"):
        final_g = din("final_g", [1, D])
    out = nc.dram_tensor("out", [S_LEN, D], F32, kind="ExternalOutput").ap()
    xs = nc.dram_tensor("xs_scr", [S_LEN, D], F32, kind="Internal").ap()
    hs = nc.dram_tensor("hs_scr", [S_LEN, D], BF16, kind="Internal").ap()

    S = Sched(nc)
    _uid = [0]

    def sb(name, shape, dt):
        _uid[0] += 1
        return nc.sbuf_tensor("%s_%d" % (name, _uid[0]), shape, dt)

    def ps(name, shape, dt):
        _uid[0] += 1
        return nc.psum_tensor("%s_%d" % (name, _uid[0]), shape, dt)

    with ExitStack() as _st:
        ident = _st.enter_context(sb("ident", [128, 128], BF16))
        identf = _st.enter_context(sb("identf", [128, 128], F32))
        ones = _st.enter_context(sb("ones", [1, 128], F32))
        modt = _st.enter_context(sb("modt", [128, 6, D], F32))
        cbc = _st.enter_context(sb("cbc", [128, KC, 128], F32))
        cact = _st.enter_context(sb("cact", [128, KC], F32))
        rowt = _st.enter_context(sb("rowt", [1, D], F32))
        rowb = _st.enter_context(sb("rowb", [1, 2, 512], F32))
        sinkbc = _st.enter_context(sb("sinkbc", [128, 16], F32))
        S.op("pool", lambda g: g.memset(identf[:], 0.0), writes=["identf"])
        S.op("pool", lambda g: g.affine_select(out=identf[:], in_=identf[:], pattern=[[-1, 128]],
                                               compare_op=ALU.not_equal, fill=1.0, base=0,
                                               channel_multiplier=1),
             reads=["identf"], writes=["identf"])
        S.op("dve", lambda v: v.tensor_copy(out=ident[:], in_=identf[:]), reads=["identf"], writes=["ident"])
        S.op("dve", lambda v: v.memset(ones[:], 1.0), writes=["ones"])
        S.dma("sp", lambda q: q.dma_start(out=cact[:], in_=cT_in), writes=["cact"])
        S.op("act", lambda a: a.activation(out=cact[:], in_=cact[:], func=AF.Silu), reads=["cact"], writes=["cact"])
        S.op("dve", lambda v: v.tensor_copy(out=cbc[:], in_=cact[:].unsqueeze(2).to_broadcast([128, KC, 128])),
             reads=["cact"], writes=["cbc"])
        if do("attn1"):
            S.dma("sp", lambda q: q.dma_start(out=sinkbc[:], in_=sw_sinks.partition_broadcast(128)), writes=["sinkbc"])

        def bcast_row(dst, row_ap, width, pst, pname):
            S.dma("sp", lambda q: q.dma_start(out=rowt[0:1, 0:width], in_=row_ap), writes=["rowt"])
            for j in range(width // 512):
                S.op("pe", lambda pe, j=j: pe.matmul(pst[:, 0:512], lhsT=ones[0:1, :], rhs=rowt[0:1, j * 512:(j + 1) * 512],
                                                     start=True, stop=True),
                     reads=["rowt", "ones"], writes=[pname])
                S.op("act", lambda a, j=j: a.activation(out=dst[:, j * 512:(j + 1) * 512], in_=pst[:, 0:512], func=AF.Copy),
                     reads=[pname], writes=["bc_dst"])

        def mod_phase(l):
            with ExitStack() as _st:
                gbc = _st.enter_context(sb("gbc", [128, 2, D], F32))
                adaw = _st.enter_context(sb("adaw", [128, 2, KC, 512], F32))
                pm = _st.enter_context(ps("pm", [128, 2, 512], F32))
                for i in range(2):
                    bcast_row(gbc[:, i, :], norm_g[l, i:i + 1, :], D, pm[:, 0, :], "pm0")
                wv = ada_w[l].rearrange("(k p) n -> p k n", p=128)
                for cg in range(12):
                    b = cg % 2
                    S.dma("sp", lambda q, cg=cg, b=b: q.dma_start(out=adaw[:, b], in_=wv[:, :, cg * 512:(cg + 1) * 512]),
                          writes=["adaw%d" % b])
                    S.dma("sp", lambda q, cg=cg, b=b: q.dma_start(out=rowb[0:1, b, :], in_=ada_b[l:l + 1, cg * 512:(cg + 1) * 512]),
                          writes=["rowb%d" % b])

                    def f(pe, cg=cg, b=b):
                        for k in range(KC):
                            pe.matmul(pm[:, b, :], lhsT=cbc[:, k, :], rhs=adaw[:, b, k, :], start=(k == 0), stop=False)
                        return pe.matmul(pm[:, b, :], lhsT=ones[0:1, :], rhs=rowb[0:1, b, :],
                                         start=False, stop=True)
                    S.op("pe", f, reads=["adaw%d" % b, "cbc", "rowb%d" % b, "ones"], writes=["pm%d" % b])
                    j, hf = cg // 2, cg % 2
                    dst = modt[:, j, hf * 512:(hf + 1) * 512]
                    if j in (1, 4):
                        gsl = gbc[:, 0 if j == 1 else 1, hf * 512:(hf + 1) * 512]
                        S.op("dve", lambda v, dst=dst, b=b, gsl=gsl: v.scalar_tensor_tensor(
                            out=dst, in0=pm[:, b, :], scalar=1.0, in1=gsl, op0=ALU.add, op1=ALU.mult),
                            reads=["pm%d" % b, "bc_dst"], writes=["modt"])
                    else:
                        S.op("act", lambda a, dst=dst, b=b: a.activation(out=dst, in_=pm[:, b, :], func=AF.Copy),
                             reads=["pm%d" % b], writes=["modt"])
                S.barrier()

        def rms_stats(xv, xt, sqj, ssq, rstd, xs_dep):
            for t in range(NT):
                b = t % 2
                S.dma("sp", lambda q, t=t, b=b: q.dma_start(out=xt[:, b, :], in_=xv[t]), reads=xs_dep, writes=["xt%d" % b])
                S.op("act", lambda a, t=t, b=b: a.activation(out=sqj, in_=xt[:, b, :], func=AF.Square, accum_out=ssq[:, t:t + 1]),
                     reads=["xt%d" % b], writes=["ssq%d" % t, "sqj"])
            S.op("dve", lambda v: v.tensor_scalar(out=rstd, in0=ssq, scalar1=1.0 / D, scalar2=EPS, op0=ALU.mult, op1=ALU.add),
                 reads=["ssq%d" % t for t in range(NT)], writes=["rstd"])
            S.op("act", lambda a: a.activation(out=rstd, in_=rstd, func=AF.Sqrt), reads=["rstd"], writes=["rstd"])
            S.op("dve", lambda v: v.reciprocal(out=rstd, in_=rstd), reads=["rstd"], writes=["rstd"])

        def attn_phase(l):
            is_na = (l == 0)
            xin = x_in if l == 0 else xs
            wqkv_d = na_wqkv if is_na else sw_wqkv
            wo_d = na_wo if is_na else sw_wo
            bias_w = NA_TTOT if is_na else 384
            with ExitStack() as _st:
                hT = _st.enter_context(sb("hT", [128, KC, S_LEN], BF16))
                attnT = _st.enter_context(sb("attnT", [128, KC, S_LEN], BF16))
                qT = _st.enter_context(sb("qT", [128, S_LEN], BF16))
                kT = _st.enter_context(sb("kT", [128, S_LEN], BF16))
                vv = _st.enter_context(sb("vv", [128, NT, 128], BF16))
                wqkv = _st.enter_context(sb("wqkv", [128, 2, KC, 3, 128], BF16))
                wo = _st.enter_context(sb("wo", [128, KC, D], BF16))
                biast = _st.enter_context(sb("biast", [128, 2, bias_w], F32))
                oall = _st.enter_context(sb("oall", [128, NT, 128], BF16))
                xt = _st.enter_context(sb("xt", [128, 2, D], F32))
                tmpn = _st.enter_context(sb("tmpn", [128, 2, D], F32))
                sqj = _st.enter_context(sb("sqj", [128, D], BF16))
                ssq = _st.enter_context(sb("ssq", [128, NT], F32))
                rstd = _st.enter_context(sb("rstd", [128, NT], F32))
                hb = _st.enter_context(sb("hb", [128, 2, D], BF16))
                ssb = _st.enter_context(sb("ssb", [128, 2, 576], F32))
                pp = _st.enter_context(sb("pp", [128, 2, 576], BF16))
                pTs = _st.enter_context(sb("pTs", [128, 2, 5, 128], BF16))
                st2 = _st.enter_context(sb("st2", [128, 8, 8], F32))
                pS = _st.enter_context(ps("pS", [128, 2, 2, 512], F32))
                pT = _st.enter_context(ps("pT", [128, 2, 8, 128], BF16))
                pVO = _st.enter_context(ps("pVO", [128, 2, 512], F32))
                pV = pVO[:, 0, :].rearrange("p (a b) -> p a b", a=4)
                pO = pVO[:, :, 0:64]
                S.dma("pool", lambda g: g.dma_start(out=wo[:], in_=wo_d.rearrange("(k p) n -> p k n", p=128)), writes=["wo"])
                xv = xin.rearrange("(t p) d -> t p d", p=128)
                xs_dep = ["xs"] if l == 1 else []
                rms_stats(xv, xt, sqj[:], ssq[:], rstd[:], xs_dep)
                for t in range(NT):
                    b = t % 2
                    S.dma("sp", lambda q, t=t, b=b: q.dma_start(out=xt[:, b, :], in_=xv[t]), reads=xs_dep, writes=["xt%d" % b])
                    S.op("dve", lambda v, t=t, b=b: v.scalar_tensor_tensor(out=tmpn[:, b, :], in0=xt[:, b, :], scalar=rstd[:, t:t + 1],
                                                                           in1=modt[:, 1, :], op0=ALU.mult, op1=ALU.mult),
                         reads=["xt%d" % b, "rstd", "modt"], writes=["tmpn%d" % b])
                    S.op("dve", lambda g, b=b: g.tensor_tensor(out=hb[:, b, :], in0=tmpn[:, b, :], in1=modt[:, 0, :], op=ALU.add),
                         reads=["tmpn%d" % b, "modt"], writes=["hb%d" % b])

                    def f(pe, b=b):
                        for k in range(KC):
                            ins = pe.transpose(out=pT[:, b, k, :], in_=hb[:, b, k * 128:(k + 1) * 128], identity=ident[:])
                        return ins
                    S.op("pe", f, reads=["hb%d" % b, "ident"], writes=["pT%d" % b])
                    S.op("act", lambda a, t=t, b=b: a.activation(out=hT[:, :, t * 128:(t + 1) * 128], in_=pT[:, b, :, :], func=AF.Copy),
                         reads=["pT%d" % b], writes=["hT"])
                wsrc = wqkv_d.rearrange("(k p) n -> p k n", p=128)
                import os
                for hp in range(int(os.environ.get("DBG_NPAIRS", "8"))):
                    wb = hp % 2
                    wres = "wqkv%d" % wb
                    if is_na:
                        for j in range(3):
                            S.dma("pool", lambda g, j=j, hp=hp, wb=wb: g.dma_start(
                                out=wqkv[:, wb, :, j, :], in_=wsrc[:, :, j * D + hp * 128: j * D + (hp + 1) * 128]), writes=[wres])
                    else:
                        kvh = hp // 2
                        S.dma("pool", lambda g, hp=hp, wb=wb: g.dma_start(
                            out=wqkv[:, wb, :, 0, :], in_=wsrc[:, :, hp * 128:(hp + 1) * 128]), writes=[wres])
                        for j in (1, 2):
                            c0 = 1024 + (j - 1) * 256 + kvh * 64
                            for hh in range(2):
                                S.dma("pool", lambda g, j=j, hh=hh, wb=wb, c0=c0: g.dma_start(
                                    out=wqkv[:, wb, :, j, hh * 64:(hh + 1) * 64], in_=wsrc[:, :, c0:c0 + 64]), writes=[wres])
                    bsrc = na_bias if is_na else sw_bias

                    def load_bias(hp_, hh_):
                        S.dma("sp", lambda q: q.dma_start(out=biast[:, hh_, :], in_=bsrc[2 * hp_ + hh_]), writes=["bias%d" % hh_])
                    if hp == 0:
                        load_bias(0, 0)
                        load_bias(0, 1)
                    for j, dstT in ((0, qT), (1, kT)):
                        for tg in range(4):
                            pi = (j * 4 + tg) % 4
                            pst = pS[:, pi // 2, pi % 2, :]

                            def f(pe, j=j, tg=tg, pst=pst, wb=wb):
                                for k in range(KC):
                                    ins = pe.matmul(pst, lhsT=wqkv[:, wb, k, j, :], rhs=hT[:, k, tg * 512:(tg + 1) * 512],
                                                    start=(k == 0), stop=(k == KC - 1))
                                return ins
                            S.op("pe", f, reads=[wres, "hT"], writes=["pS%d" % pi])
                            S.op("act", lambda a, j=j, tg=tg, pst=pst, dstT=dstT: a.activation(
                                out=dstT[:, tg * 512:(tg + 1) * 512], in_=pst, func=AF.Copy, scale=(0.125 if j == 0 else 1.0)),
                                reads=["pS%d" % pi], writes=["qT" if j == 0 else "kT"])
                    for t4 in range(4):
                        def f(pe, t4=t4, wb=wb):
                            for tt in range(4):
                                t = t4 * 4 + tt
                                for k in range(KC):
                                    ins = pe.matmul(pV[:, tt, :], lhsT=hT[:, k, t * 128:(t + 1) * 128], rhs=wqkv[:, wb, k, 2, :],
                                                    start=(k == 0), stop=(k == KC - 1))
                            return ins
                        S.op("pe", f, reads=[wres, "hT"], writes=["pO0"])
                        S.op("dve", lambda v, t4=t4: v.tensor_copy(out=vv[:, t4 * 4:(t4 + 1) * 4, :], in_=pV),
                             reads=["pO0"], writes=["vv"])
                    iters = []
                    for hh in range(2):
                        for qb in range(NT):
                            if is_na:
                                r0 = 2 * qb
                                if r0 <= 2:
                                    krow, tt_ = 0, r0 // 2
                                elif r0 >= 28:
                                    krow, tt_ = 24, 3 + (r0 - 28) // 2
                                else:
                                    krow, tt_ = r0 - 4, 2
                                nk = NA_TW[tt_]
                                boff = NA_TOFF[tt_]
                                ks = krow * 64
                            else:
                                t_lo, t_hi = max(qb - 1, 0), min(qb + 1, NT - 1)
                                nk = (t_hi - t_lo + 1) * 128
                                ks = t_lo * 128
                                boff = 128 if qb == 0 else 0
                            iters.append(dict(hh=hh, qb=qb, nk=nk, boff=boff, ks=ks, half=nk // 2, kt0=ks // 128,
                                              nfull=nk // 128, rem=nk % 128, h=2 * hp + hh, hsl=slice(hh * 64, (hh + 1) * 64)))

                    def stA(j, it):
                        sb_ = j % 2

                        def f(pe):
                            for pc in range(2):
                                ins = pe.matmul(pS[:, sb_, pc, 0:it["half"]], lhsT=qT[it["hsl"], it["qb"] * 128:(it["qb"] + 1) * 128],
                                                rhs=kT[it["hsl"], it["ks"] + pc * it["half"]: it["ks"] + (pc + 1) * it["half"]],
                                                start=True, stop=True)
                            return ins
                        S.op("pe", f, reads=["qT", "kT"], writes=["pS%d" % (2 * sb_), "pS%d" % (2 * sb_ + 1)])

                    def stB(j, it):
                        sb_ = j % 2
                        nk, half, boff, hh, h = it["nk"], it["half"], it["boff"], it["hh"], it["h"]
                        stt = st2[:, j % 8, :]
                        sres = "st2_%d" % (j % 8)
                        S.op("dve", lambda v: v.tensor_tensor(
                            out=ssb[:, sb_, 0:nk].rearrange("p (c n) -> p c n", c=2), in0=pS[:, sb_, :, 0:half],
                            in1=biast[:, hh, boff:boff + nk].rearrange("p (c n) -> p c n", c=2), op=ALU.add),
                            reads=["pS%d" % (2 * sb_), "pS%d" % (2 * sb_ + 1), "bias%d" % hh], writes=["ssb%d" % sb_])
                        if is_na:
                            S.op("dve", lambda v: v.tensor_reduce(out=stt[:, 0:1], in_=ssb[:, sb_, 0:nk], axis=AX.X, op=ALU.max, negate=True),
                                 reads=["ssb%d" % sb_], writes=[sres])
                        else:
                            S.op("dve", lambda v: v.tensor_reduce(out=stt[:, 4:5], in_=ssb[:, sb_, 0:nk], axis=AX.X, op=ALU.max),
                                 reads=["ssb%d" % sb_], writes=[sres])
                            S.op("dve", lambda v: v.tensor_scalar(out=stt[:, 0:1], in0=stt[:, 4:5], scalar1=sinkbc[:, h:h + 1],
                                                                  scalar2=-1.0, op0=ALU.max, op1=ALU.mult),
                                 reads=[sres, "sinkbc"], writes=[sres])

                    def stC(j, it):
                        sb_ = j % 2
                        nk, h = it["nk"], it["h"]
                        stt = st2[:, j % 8, :]
                        sres = "st2_%d" % (j % 8)
                        S.op("act", lambda a: a.activation(out=pp[:, sb_, 0:nk], in_=ssb[:, sb_, 0:nk], func=AF.Exp, bias=stt[:, 0:1],
                                                           scale=1.0, accum_out=stt[:, 1:2]),
                             reads=["ssb%d" % sb_, sres], writes=["pp%d" % sb_, sres + "s"])
                        if not is_na:
                            S.op("act", lambda a: a.activation(out=stt[:, 5:6], in_=sinkbc[:, h:h + 1], func=AF.Exp, bias=stt[:, 0:1], scale=1.0),
                                 reads=[sres, "sinkbc"], writes=[sres + "e"])

                    def stD(j, it):
                        sb_ = j % 2
                        nfull, rem = it["nfull"], it["rem"]
                        stt = st2[:, j % 8, :]
                        sres = "st2_%d" % (j % 8)

                        def f(pe):
                            for c in range(nfull):
                                ins = pe.transpose(out=pT[:, sb_, c, :], in_=pp[:, sb_, c * 128:(c + 1) * 128], identity=ident[:])
                            if rem:
                                ins = pe.transpose(out=pT[0:rem, sb_, nfull, :], in_=pp[:, sb_, nfull * 128: nfull * 128 + rem], identity=ident[:])
                            return ins
                        S.op("pe", f, reads=["pp%d" % sb_, "ident"], writes=["pT%d" % sb_])
                        if not is_na:
                            S.op("dve", lambda v: v.tensor_tensor(out=stt[:, 1:2], in0=stt[:, 1:2], in1=stt[:, 5:6], op=ALU.add),
                                 reads=[sres + "s", sres + "e"], writes=[sres + "s"])
                        S.op("dve", lambda v: v.reciprocal(out=stt[:, 2:3], in_=stt[:, 1:2]), reads=[sres + "s"], writes=[sres + "r"])

                    def stE(j, it):
                        sb_ = j % 2
                        nfull, rem = it["nfull"], it["rem"]
                        if (j // 2) % 2 == 0:
                            S.op("act", lambda a: a.activation(out=pTs[:, sb_, 0:nfull, :], in_=pT[:, sb_, 0:nfull, :], func=AF.Copy),
                                 reads=["pT%d" % sb_], writes=["pTs%d" % sb_])
                            if rem:
                                S.op("act", lambda a: a.activation(out=pTs[0:rem, sb_, nfull, :], in_=pT[0:rem, sb_, nfull, :], func=AF.Copy),
                                     reads=["pT%d" % sb_], writes=["pTs%d" % sb_])
                        else:
                            S.op("dve", lambda v: v.tensor_copy(out=pTs[:, sb_, 0:nfull, :], in_=pT[:, sb_, 0:nfull, :]),
                                 reads=["pT%d" % sb_], writes=["pTs%d" % sb_])
                            if rem:
                                S.op("dve", lambda v: v.tensor_copy(out=pTs[0:rem, sb_, nfull, :], in_=pT[0:rem, sb_, nfull, :]),
                                     reads=["pT%d" % sb_], writes=["pTs%d" % sb_])

                    def stF(j, it):
                        sb_ = j % 2
                        nfull, rem, kt0, hsl = it["nfull"], it["rem"], it["kt0"], it["hsl"]

                        def f(pe):
                            tot = nfull + (1 if rem else 0)
                            for c in range(nfull):
                                ins = pe.matmul(pO[:, sb_, :], lhsT=pTs[:, sb_, c, :], rhs=vv[:, kt0 + c, hsl], start=(c == 0), stop=(c == tot - 1))
                            if rem:
                                ins = pe.matmul(pO[:, sb_, :], lhsT=pTs[0:rem, sb_, nfull, :], rhs=vv[0:rem, kt0 + nfull, hsl], start=False, stop=True)
                            return ins
                        S.op("pe", f, reads=["pTs%d" % sb_, "vv"], writes=["pO%d" % sb_])

                    def stG(j, it):
                        sb_ = j % 2
                        stt = st2[:, j % 8, :]
                        sres = "st2_%d" % (j % 8)
                        S.op("act", lambda a: a.activation(out=oall[:, it["qb"], it["hsl"]], in_=pO[:, sb_, :], func=AF.Copy, scale=stt[:, 2:3]),
                             reads=["pO%d" % sb_, sres + "r"], writes=["oall"])

                    stages = [stA, stB, stC, stD, stE, stF, stG][:int(os.environ.get("DBG_NST", "7"))]
                    n_it = min(len(iters), int(os.environ.get("DBG_NIT", "99")))
                    SKEW = True
                    for step in range(n_it + (len(stages) - 1 if SKEW else 0)):
                        for si in (range(len(stages) - 1, -1, -1) if SKEW else range(len(stages))):
                            j = step - si if SKEW else step
                            if 0 <= j < n_it:
                                stages[si](j, iters[j])
                        if hp + 1 < 8 and step == NT:
                            load_bias(hp + 1, 0)
                    if hp + 1 < 8:
                        load_bias(hp + 1, 1)
                    for t4 in range(4):
                        b = t4 % 2

                        def f(pe, t4=t4, b=b):
                            for tt in range(4):
                                ins = pe.transpose(out=pT[:, b, tt, :], in_=oall[:, t4 * 4 + tt, :], identity=ident[:])
                            return ins
                        S.op("pe", f, reads=["oall", "ident"], writes=["pT%d" % b])
                        S.op("dve", lambda v, t4=t4, b=b, hp=hp: v.tensor_copy(
                            out=attnT[:, hp, t4 * 512:(t4 + 1) * 512].rearrange("p (t n) -> p t n", t=4), in_=pT[:, b, 0:4, :]),
                            reads=["pT%d" % b], writes=["attnT"])
                xo = xs.rearrange("(t p) d -> t p d", p=128)
                for t in range(NT):
                    b = t % 2
                    S.dma("sp", lambda q, t=t, b=b: q.dma_start(out=xt[:, b, :], in_=xv[t]), reads=xs_dep, writes=["xt%d" % b])

                    def f(pe, t=t, b=b):
                        for n in range(2):
                            for k in range(KC):
                                ins = pe.matmul(pS[:, b, n, :], lhsT=attnT[:, k, t * 128:(t + 1) * 128], rhs=wo[:, k, n * 512:(n + 1) * 512],
                                                start=(k == 0), stop=(k == KC - 1))
                        return ins
                    S.op("pe", f, reads=["attnT", "wo"], writes=["pS%d" % (2 * b), "pS%d" % (2 * b + 1)])
                    S.op("dve", lambda v, b=b: v.tensor_tensor(out=tmpn[:, b, :].rearrange("p (n c) -> p n c", n=2), in0=pS[:, b, :, :],
                                                                in1=modt[:, 2, :].rearrange("p (n c) -> p n c", n=2), op=ALU.mult),
                         reads=["pS%d" % (2 * b), "pS%d" % (2 * b + 1), "modt"], writes=["tmpn%d" % b])
                    S.op("dve", lambda g, b=b: g.tensor_tensor(out=xt[:, b, :], in0=xt[:, b, :], in1=tmpn[:, b, :], op=ALU.add),
                         reads=["tmpn%d" % b, "xt%d" % b], writes=["xt%d" % b])
                    S.dma("sp", lambda q, t=t, b=b: q.dma_start(out=xo[t], in_=xt[:, b, :]), reads=["xt%d" % b], writes=["xs"])
                S.barrier()

        def moe_phase(l):
            RING = 14
            with sb("ring", [128, RING, 4096], BF16) as ring:
                chunk_no = [0]

                def load_chunk(kind, e, i):
                    slot = chunk_no[0] % RING
                    chunk_no[0] += 1
                    res = "ring%d" % slot
                    if kind == "down":
                        src = w_down[l, e, i * 512:(i + 1) * 512, :].rearrange("(j p) n -> p j n", p=128)
                        dst = ring[:, slot, :].rearrange("p (j n) -> p j n", j=4)
                    else:
                        wsrc = w_gate if kind == "gate" else w_up
                        src = wsrc[l, e, :, i * 512:(i + 1) * 512].rearrange("(k p) n -> p k n", p=128)
                        dst = ring[:, slot, :].rearrange("p (k n) -> p k n", k=KC)
                    S.dma("pool", lambda g: g.dma_start(out=dst, in_=src), writes=[res])
                    return slot

                def load_expert(e):
                    sl = {}
                    for fg in range(4):
                        sl[("gate", fg)] = load_chunk("gate", e, fg)
                        sl[("up", fg)] = load_chunk("up", e, fg)
                    for i in range(4):
                        sl[("down", i)] = load_chunk("down", e, i)
                    return sl

                slots0 = load_expert(0)
                with ExitStack() as _st:
                    idxT = _st.enter_context(sb("idxT", [128, 2, NE], U32))
                    valsT = _st.enter_context(sb("valsT", [128, 2, NE], F32))
                    with ExitStack() as _st:
                        xt = _st.enter_context(sb("xt", [128, 2, D], F32))
                        tmpn = _st.enter_context(sb("tmpn", [128, 2, D], F32))
                        sqj = _st.enter_context(sb("sqj", [128, D], BF16))
                        ssq = _st.enter_context(sb("ssq", [128, NT], F32))
                        rstd = _st.enter_context(sb("rstd", [128, NT], F32))
                        rst = _st.enter_context(sb("rst", [128, 3, NT], F32))
                        lg2 = _st.enter_context(sb("lg2", [128, 2, NE], F32))
                        h2f = _st.enter_context(sb("h2f", [128, 2, D], F32))
                        h2b = _st.enter_context(sb("h2b", [128, 2, D], BF16))
                        h2T = _st.enter_context(sb("h2T", [128, 2, KC, 128], F32))
                        wr = _st.enter_context(sb("wr", [128, KC, NE], F32))
                        stat = _st.enter_context(sb("stat", [128, 8], F32))
                        lg = _st.enter_context(sb("lg", [128, 2, NE], F32))
                        cur = _st.enter_context(sb("cur", [16, 2, S_LEN], F32))
                        vals = _st.enter_context(sb("vals", [16, CAP], F32))
                        idx = _st.enter_context(sb("idx", [16, CAP], U32))
                        idxf = _st.enter_context(sb("idxf", [16, CAP], F32))
                        pF = _st.enter_context(ps("pF", [128, 2, 2, 4, 128], F32))
                        pL = _st.enter_context(ps("pL", [128, 2, 512], F32))
                        pA = _st.enter_context(ps("pA", [128, 2, 512], F32))
                        pI = pA[:, 0, 0:2 * NE].rearrange("p (a b) -> p a b", a=2)
                        S.dma("sp", lambda q: q.dma_start(out=wr[:], in_=w_router[l].rearrange("(k p) n -> p k n", p=128)), writes=["wr"])
                        xv = xs.rearrange("(t p) d -> t p d", p=128)
                        hv = hs.rearrange("(t p) d -> t p d", p=128)
                        rms_stats(xv, xt, sqj[:], ssq[:], rstd[:], ["xs"])

                        def q0(t):
                            b = t % 2
                            S.dma("sp", lambda q: q.dma_start(out=xt[:, b, :], in_=xv[t]), reads=["xs"], writes=["xt%d" % b])

                        def q1(t):
                            b = t % 2
                            S.op("dve", lambda v: v.scalar_tensor_tensor(out=tmpn[:, b, :], in0=xt[:, b, :], scalar=rstd[:, t:t + 1],
                                                                         in1=modt[:, 4, :], op0=ALU.mult, op1=ALU.mult),
                                 reads=["xt%d" % b, "rstd", "modt"], writes=["tmpn%d" % b])

                        def q2(t):
                            b = t % 2
                            S.op("dve", lambda v: v.tensor_tensor(out=h2f[:, b, :], in0=tmpn[:, b, :], in1=modt[:, 3, :], op=ALU.add),
                                 reads=["tmpn%d" % b, "modt"], writes=["h2f%d" % b])

                        def q3(t):
                            b = t % 2
                            S.op("act", lambda a: a.activation(out=h2b[:, b, :], in_=h2f[:, b, :], func=AF.Copy),
                                 reads=["h2f%d" % b], writes=["h2b%d" % b])
                            for g2 in range(2):
                                def f(pe, g2=g2):
                                    for kk in range(4):
                                        k = g2 * 4 + kk
                                        ins = pe.transpose(out=pF[:, b, g2, kk, :], in_=h2f[:, b, k * 128:(k + 1) * 128], identity=identf[:])
                                    return ins
                                S.op("pe", f, reads=["h2f%d" % b, "identf"], writes=["pF%d_%d" % (b, g2)])

                        def q4(t):
                            b = t % 2
                            S.dma("sp", lambda q: q.dma_start(out=hv[t], in_=h2b[:, b, :]), reads=["h2b%d" % b], writes=["hs"])
                            for g2 in range(2):
                                S.op("act", lambda a, g2=g2: a.activation(out=h2T[:, b, g2 * 4:(g2 + 1) * 4, :], in_=pF[:, b, g2, :, :], func=AF.Copy),
                                     reads=["pF%d_%d" % (b, g2)], writes=["h2T%d" % b])

                        def q5(t):
                            b = t % 2

                            def f(pe):
                                for k in range(KC):
                                    ins = pe.matmul(pL[:, b, 0:NE], lhsT=h2T[:, b, k, :], rhs=wr[:, k, :], start=(k == 0), stop=(k == KC - 1))
                                return ins
                            S.op("pe", f, reads=["h2T%d" % b, "wr"], writes=["pL%d" % b])

                        def q6(t):
                            b = t % 2
                            S.op("dve", lambda v: v.tensor_reduce(out=rst[:, 0, t:t + 1], in_=pL[:, b, 0:NE], axis=AX.X, op=ALU.max, negate=True),
                                 reads=["pL%d" % b], writes=["rm%d" % t])

                        def q7(t):
                            b = t % 2
                            S.op("act", lambda a: a.activation(out=lg[:, b, :], in_=pL[:, b, 0:NE], func=AF.Exp, bias=rst[:, 0, t:t + 1], scale=1.0,
                                                               accum_out=rst[:, 1, t:t + 1]),
                                 reads=["pL%d" % b, "rm%d" % t], writes=["lg%d" % b, "rs%d" % t])

                        def q8(t):
                            b = t % 2
                            S.op("dve", lambda v: v.reciprocal(out=rst[:, 2, t:t + 1], in_=rst[:, 1, t:t + 1]), reads=["rs%d" % t], writes=["rr%d" % t])
                            S.op("dve", lambda v: v.tensor_scalar(out=lg2[:, b, :], in0=lg[:, b, :], scalar1=rst[:, 2, t:t + 1], scalar2=None,
                                                                  op0=ALU.mult), reads=["lg%d" % b, "rr%d" % t], writes=["lg2_%d" % b])

                        def q9(t):
                            b = t % 2
                            S.op("pe", lambda pe: pe.transpose(out=pA[0:NE, b, 0:128], in_=lg2[:, b, :], identity=identf[:]),
                                 reads=["lg2_%d" % b, "identf"], writes=["pA%d" % b])

                        def q10(t):
                            b = t % 2
                            S.op("act", lambda a: a.activation(out=cur[:, 0, t * 128:(t + 1) * 128], in_=pA[0:NE, b, 0:128], func=AF.Copy),
                                 reads=["pA%d" % b], writes=["cur0"])

                        rstages = [q0, q1, q2, q3, q4, q5, q6, q7, q8, q9, q10]
                        for step in range(NT + len(rstages) - 1):
                            for si in range(len(rstages) - 1, -1, -1):
                                t = step - si
                                if 0 <= t < NT:
                                    rstages[si](t)
                        for r in range(CAP // 8):
                            c, n = r % 2, (r + 1) % 2
                            rs_ = slice(r * 8, (r + 1) * 8)
                            S.op("dve", lambda v, rs_=rs_, c=c: v.max(out=vals[:, rs_], in_=cur[:, c, :]), reads=["cur%d" % c], writes=["vals"])
                            S.op("dve", lambda v, rs_=rs_, c=c: v.max_index(out=idx[:, rs_], in_max=vals[:, rs_], in_values=cur[:, c, :]),
                                 reads=["cur%d" % c, "vals"], writes=["idx"])
                            S.op("dve", lambda v, rs_=rs_, c=c, n=n: v.match_replace(out=cur[:, n, :], in_to_replace=vals[:, rs_],
                                                                                    in_values=cur[:, c, :], imm_value=-1.0),
                                 reads=["cur%d" % c, "vals"], writes=["cur%d" % n])
                        S.op("dve", lambda v: v.tensor_copy(out=idxf[:], in_=idx[:]), reads=["idx"], writes=["idxf"])
                        for src_t, dst_t, nm in ((idxf, idxT, "idxT"), (vals, valsT, "valsT")):
                            def f(pe, src_t=src_t):
                                for hh in range(2):
                                    ins = pe.transpose(out=pI[:, hh, :], in_=src_t[:, hh * 128:(hh + 1) * 128], identity=identf[0:16, 0:16])
                                return ins
                            S.op("pe", f, reads=["idxf", "vals", "identf"], writes=["pA0"])
                            S.op("dve", lambda v, dst_t=dst_t: v.tensor_copy(out=dst_t[:], in_=pI), reads=["pA0"], writes=[nm])
                        S.barrier()
                    with ExitStack() as _st:
                        xg = _st.enter_context(sb("xg", [128, 2, 2, D], BF16))
                        xgT = _st.enter_context(sb("xgT", [128, 2, KC, CAP], BF16))
                        actT = _st.enter_context(sb("actT", [128, 2, 16, CAP], BF16))
                        sg = _st.enter_context(sb("sg", [128, 2, CAP], F32))
                        ys = _st.enter_context(sb("ys", [128, 2, D], F32))
                        pX = _st.enter_context(ps("pX", [128, 2, KC, 128], BF16))
                        pG = _st.enter_context(ps("pG", [128, 2, 2, CAP], F32))
                        pY = _st.enter_context(ps("pY", [128, 2, 2, 512], F32))
                        slots = slots0

                        def gather(e):
                            eb = e % 2
                            for hf in range(2):
                                S.dma("pool", lambda g, e=e, hf=hf, eb=eb: g.indirect_dma_start(
                                    out=xg[:, eb, hf, :], out_offset=None, in_=hs,
                                    in_offset=bass.IndirectOffsetOnAxis(ap=idxT[:, hf, e:e + 1], axis=0)),
                                    reads=["idxT", "hs"], writes=["xg%d_%d" % (eb, hf)])
                        gather(0)
                        for e in range(NE):
                            eb = e % 2
                            for hf in range(2):
                                def f(pe, eb=eb, hf=hf):
                                    for k in range(KC):
                                        ins = pe.transpose(out=pX[:, hf, k, :], in_=xg[:, eb, hf, k * 128:(k + 1) * 128], identity=ident[:])
                                    return ins
                                S.op("pe", f, reads=["xg%d_%d" % (eb, hf), "ident"], writes=["pX%d" % hf])
                                S.op("act", lambda a, eb=eb, hf=hf: a.activation(out=xgT[:, eb, :, hf * 128:(hf + 1) * 128], in_=pX[:, hf, :, :], func=AF.Copy),
                                     reads=["pX%d" % hf], writes=["xgT%d" % eb])
                            fi_n = 0
                            for fg in range(4):
                                sg_ = slots[("gate", fg)]
                                su_ = slots[("up", fg)]
                                gch = ring[:, sg_, :].rearrange("p (k n) -> p k n", k=KC)
                                uch = ring[:, su_, :].rearrange("p (k n) -> p k n", k=KC)
                                for fs in range(4):
                                    fi = fg * 4 + fs
                                    pb = fi % 2

                                    def f(pe, gch=gch, uch=uch, fs=fs, pb=pb, eb=eb):
                                        for j, ch in ((0, gch), (1, uch)):
                                            for k in range(KC):
                                                ins = pe.matmul(pG[:, pb, j, :], lhsT=ch[:, k, fs * 128:(fs + 1) * 128], rhs=xgT[:, eb, k, :],
                                                                start=(k == 0), stop=(k == KC - 1))
                                        return ins
                                    S.op("pe", f, reads=["ring%d" % sg_, "ring%d" % su_, "xgT%d" % eb], writes=["pG%d" % pb])
                                    S.op("act", lambda a, pb=pb: a.activation(out=sg[:, pb, :], in_=pG[:, pb, 0, :], func=AF.Silu),
                                         reads=["pG%d" % pb], writes=["sg%d" % pb])
                                    S.op("dve", lambda v, pb=pb, fi=fi, eb=eb: v.tensor_tensor(out=actT[:, eb, fi, :], in0=sg[:, pb, :], in1=pG[:, pb, 1, :],
                                                                                              op=ALU.mult),
                                         reads=["sg%d" % pb, "pG%d" % pb], writes=["actT%d" % eb])
                            for tt in range(2):
                                def f(pe, tt=tt, eb=eb, slots=slots):
                                    for n in range(2):
                                        for fi in range(16):
                                            dch = ring[:, slots[("down", fi // 4)], :].rearrange("p (j n) -> p j n", j=4)
                                            ins = pe.matmul(pY[:, tt, n, :], lhsT=actT[:, eb, fi, tt * 128:(tt + 1) * 128],
                                                            rhs=dch[:, fi % 4, n * 512:(n + 1) * 512], start=(fi == 0), stop=(fi == 15))
                                    return ins
                                S.op("pe", f, reads=["actT%d" % eb] + ["ring%d" % slots[("down", i)] for i in range(4)],
                                     writes=["pY%d" % tt])
                                S.op("dve", lambda v, tt=tt, e=e: v.scalar_tensor_tensor(
                                    out=ys[:, tt, :].rearrange("p (n c) -> p n c", n=2), in0=pY[:, tt, :, :], scalar=valsT[:, tt, e:e + 1],
                                    in1=modt[:, 5, :].rearrange("p (n c) -> p n c", n=2), op0=ALU.mult, op1=ALU.mult),
                                    reads=["pY%d" % tt, "valsT", "modt"], writes=["ys%d" % tt])
                            if e + 1 < NE:
                                slots_next = load_expert(e + 1)
                                gather(e + 1)
                            for tt in range(2):
                                S.dma("pool", lambda g, tt=tt, e=e: g.indirect_dma_start(
                                    out=xs, out_offset=bass.IndirectOffsetOnAxis(ap=idxT[:, tt, e:e + 1], axis=0),
                                    in_=ys[:, tt, :], in_offset=None, compute_op=ALU.add),
                                    reads=["ys%d" % tt, "idxT", "xs"], writes=["xs"])
                            if e + 1 < NE:
                                slots = slots_next
                        S.barrier()

        def final_phase():
            with ExitStack() as _st:
                fgb = _st.enter_context(sb("fgb", [128, D], F32))
                xt = _st.enter_context(sb("xt", [128, 2, D], F32))
                sqj = _st.enter_context(sb("sqj", [128, D], BF16))
                ssq = _st.enter_context(sb("ssq", [128, NT], F32))
                rstd = _st.enter_context(sb("rstd", [128, NT], F32))
                ot = _st.enter_context(sb("ot", [128, 2, D], F32))
                pm = _st.enter_context(ps("pm", [128, 512], F32))
                bcast_row(fgb[:], final_g, D, pm[:], "pmf")
                xv = xs.rearrange("(t p) d -> t p d", p=128)
                ov = out.rearrange("(t p) d -> t p d", p=128)
                rms_stats(xv, xt, sqj[:], ssq[:], rstd[:], ["xs"])
                for t in range(NT):
                    b = t % 2
                    S.dma("sp", lambda q, t=t, b=b: q.dma_start(out=xt[:, b, :], in_=xv[t]), reads=["xs"], writes=["xt%d" % b])
                    S.op("dve", lambda v, t=t, b=b: v.scalar_tensor_tensor(out=ot[:, b, :], in0=xt[:, b, :], scalar=rstd[:, t:t + 1], in1=fgb[:],
                                                                           op0=ALU.mult, op1=ALU.mult),
                         reads=["xt%d" % b, "rstd", "bc_dst"], writes=["ot%d" % b])
                    S.dma("sp", lambda q, t=t, b=b: q.dma_start(out=ov[t], in_=ot[:, b, :]), reads=["ot%d" % b], writes=["out"])

        def copy_out():
            with sb("xt", [128, 2, D], F32) as xt:
                xv = xs.rearrange("(t p) d -> t p d", p=128)
                ov = out.rearrange("(t p) d -> t p d", p=128)
                for t in range(NT):
                    b = t % 2
                    S.dma("sp", lambda q, t=t, b=b: q.dma_start(out=xt[:, b, :], in_=xv[t]), reads=["xs"], writes=["xt%d" % b])
                    S.dma("sp", lambda q, t=t, b=b: q.dma_start(out=ov[t], in_=xt[:, b, :]), reads=["xt%d" % b], writes=["out"])

        for l in range(2):
            if not do("attn%d" % l):
                break
            mod_phase(l)
            attn_phase(l)
            if do("moe%d" % l) and not skip_moe:
                moe_phase(l)
        if do("final"):
            final_phase()
        else:
            copy_out()
        for e in Sched.ENG:
            S.wait_all_on(e)
    S.close()
    return nc, S


def _t5_buckets(rel):
    half, max_exact = 16, 8
    n = np.abs(rel)
    large = max_exact + (np.log(np.maximum(n, 1) / max_exact) / np.log(128 / max_exact) * (half - max_exact)).astype(np.int32)
    large = np.minimum(large, half - 1)
    return (rel > 0).astype(np.int32) * half + np.where(n < max_exact, n, large)


def _na_bias_tables(rpb):
    out = np.full((16, 128, NA_TTOT), NEG, np.float32)
    p = np.arange(128)
    rq, cq = p // 64, p % 64
    for tt_, r0 in enumerate([0, 2, 10, 28, 30]):
        if r0 <= 2:
            krow0 = 0
        elif r0 >= 28:
            krow0 = 24
        else:
            krow0 = r0 - 4
        nk = NA_TW[tt_]
        j = np.arange(nk)
        krow = krow0 + j // 64
        kcol = j % 64
        r = r0 + rq
        rs = np.clip(r - 4, 0, 24)
        cstart = np.clip(cq - 8, 0, 48)
        ok = ((krow[None, :] >= rs[:, None]) & (krow[None, :] < rs[:, None] + 8)
              & (kcol[None, :] >= cstart[:, None]) & (kcol[None, :] < cstart[:, None] + 16))
        dr = np.clip(krow[None, :] - r[:, None] + 7, 0, 14)
        dc = np.clip(kcol[None, :] - cq[:, None] + 15, 0, 30)
        vals = rpb[:, dr, dc]
        blk = np.where(ok[None], vals, np.float32(NEG)).astype(np.float32)
        out[:, :, NA_TOFF[tt_]:NA_TOFF[tt_] + nk] = blk
    return out


def _sw_bias_table(t5):
    rel = (np.arange(384)[None, :] - 128) - np.arange(128)[:, None]
    b = np.transpose(t5[_t5_buckets(rel)], (2, 0, 1))
    return np.where((np.abs(rel) <= 128)[None], b, np.float32(NEG)).astype(np.float32)


def make_in_maps(inputs, cores, stop_after="final", skip_moe=False):
    order = ["attn0", "moe0", "attn1", "moe1", "final"]
    do = lambda st: order.index(st) <= order.index(stop_after)
    f32 = lambda a: np.ascontiguousarray(np.asarray(a, dtype=np.float32))
    shared = {
        "ada_w": f32(inputs["ada_w"]), "ada_b": f32(inputs["ada_b"]), "norm_g": f32(inputs["norm_g"]),
        "na_w_qkv": f32(inputs["na_w_qkv"][0]), "na_w_o": f32(inputs["na_w_o"][0]),
        "na_bias": _na_bias_tables(f32(inputs["na_rpb"][0])),
    }
    if do("attn1"):
        shared.update({"sw_w_qkv": f32(inputs["sw_w_qkv"][0]), "sw_w_o": f32(inputs["sw_w_o"][0]),
                       "sw_bias": _sw_bias_table(f32(inputs["t5_bias"])), "sw_sinks": f32(inputs["sw_sinks"]).reshape(1, 16)})
    n_moe = 0 if skip_moe else (2 if do("moe1") else (1 if do("moe0") else 0))
    if n_moe:
        shared.update({"moe_w_router": f32(inputs["moe_w_router"][:n_moe]), "moe_w_gate": f32(inputs["moe_w_gate"][:n_moe]),
                       "moe_w_up": f32(inputs["moe_w_up"][:n_moe]), "moe_w_down": f32(inputs["moe_w_down"][:n_moe])})
    if do("final"):
        shared["final_g"] = f32(inputs["final_g"]).reshape(1, D)
    x = f32(inputs["x"])
    c = f32(inputs["c"])
    maps = []
    for b in cores:
        m = dict(shared)
        m["x"] = np.ascontiguousarray(x[b])
        m["cT"] = np.ascontiguousarray(c[b].reshape(KC, 128).T)
        maps.append(m)
    return maps


_PROG = {}


def kernel(**inputs):
    if "final" not in _PROG:
        _PROG["final"] = build_program("final")[0]
    nc = _PROG["final"]
    cores = list(range(8))
    in_maps = make_in_maps(inputs, cores)
    res = run_bass_kernel_spmd(nc, in_maps, core_ids=cores)
    return np.stack([np.asarray(r["out"], dtype=np.float32) for r in res.results], axis=0)
```
